# Optimizing a Trainium2 kernel written in Bass

```python
import math
import jax, jax.numpy as jnp
from jax import lax
import numpy as np

D_MODEL = 1024
BATCH = 8
SEQ = 2048
DEPTH = 1

PLE_DIM = 256
EPS = 1e-6
NEG_INF = -1e30
N_Q_HEADS = 8
N_KV_HEADS = 2
HEAD_DIM = 64
Q_PER_KV = N_Q_HEADS // N_KV_HEADS
WINDOW = 128
BLOCK = 128
LRU_WIDTH = D_MODEL
LRU_BLOCKS = 8
LRU_BLOCK_DIM = LRU_WIDTH // LRU_BLOCKS
CONV_WIDTH = 4
LRU_C = 8.0
PEER_HEADS = 8
PEER_KEYS = 128
PEER_N_EXPERTS = PEER_KEYS * PEER_KEYS
PEER_QDIM = 256
PEER_HALF = PEER_QDIM // 2
PEER_TOPK = 16
PEER_CHUNK = 128
Q_W = N_Q_HEADS * HEAD_DIM
KV_W = N_KV_HEADS * HEAD_DIM
SPLIT_POINTS = (Q_W, Q_W + KV_W, Q_W + 2 * KV_W, Q_W + 2 * KV_W + LRU_WIDTH,
                Q_W + 2 * KV_W + 2 * LRU_WIDTH, Q_W + 2 * KV_W + 2 * LRU_WIDTH + D_MODEL)
IN_WIDTH = Q_W + 2 * KV_W + 2 * LRU_WIDTH + 2 * D_MODEL

kernel_name = "hybrid_swa_rglru_peer_block"


def rms_norm(x, g):
    xf = x.astype(jnp.float32)
    y = xf * lax.rsqrt(jnp.mean(xf * xf, axis=-1, keepdims=True) + EPS)
    return (y * g.astype(jnp.float32)).astype(x.dtype)


def sliding_window_attention(q, k, v, sink):
    B, S = q.shape[0], q.shape[1]
    nb = S // BLOCK
    qb = q.reshape(B, nb, BLOCK, N_KV_HEADS, Q_PER_KV, HEAD_DIM)
    kb = k.reshape(B, nb, BLOCK, N_KV_HEADS, HEAD_DIM)
    vb = v.reshape(B, nb, BLOCK, N_KV_HEADS, HEAD_DIM)
    pad = ((0, 0), (1, 0), (0, 0), (0, 0), (0, 0))
    kk = jnp.concatenate([jnp.pad(kb, pad)[:, :-1], kb], axis=2)
    vv = jnp.concatenate([jnp.pad(vb, pad)[:, :-1], vb], axis=2)
    scores = jnp.einsum('bnqgrd,bnkgd->bgrnqk', qb, kk).astype(jnp.float32) * (HEAD_DIM ** -0.5)
    qi = jnp.arange(BLOCK)
    kj = jnp.arange(2 * BLOCK)
    dist = (qi[:, None] + BLOCK - kj[None, :]).astype(jnp.float32)
    key_pos = jnp.arange(nb)[:, None] * BLOCK + kj[None, :] - BLOCK
    mask = ((dist >= 0) & (dist < WINDOW))[None] & (key_pos >= 0)[:, None, :]
    slopes = (2.0 ** (-8.0 * jnp.arange(1, N_Q_HEADS + 1, dtype=jnp.float32) / N_Q_HEADS))
    slopes = slopes.reshape(N_KV_HEADS, Q_PER_KV)[:, :, None, None, None]
    scores = jnp.where(mask, scores - slopes * dist, NEG_INF)
    sink_logit = sink.astype(jnp.float32).reshape(N_KV_HEADS, Q_PER_KV)[None, :, :, None, None, None]
    sink_logit = jnp.broadcast_to(sink_logit, scores.shape[:-1] + (1,))
    probs = jax.nn.softmax(jnp.concatenate([scores, sink_logit], axis=-1), axis=-1)[..., :-1]
    out = jnp.einsum('bgrnqk,bnkgd->bnqgrd', probs.astype(v.dtype), vv)
    return out.reshape(B, S, Q_W)


def causal_depthwise_conv(x, w, b):
    S = x.shape[1]
    xp = jnp.pad(x, ((0, 0), (CONV_WIDTH - 1, 0), (0, 0)))
    y = b + xp[:, 0:S] * w[0]
    for tap in range(1, CONV_WIDTH):
        y = y + xp[:, tap:tap + S] * w[tap]
    return y


def block_diag_linear(x, w, b):
    B, S = x.shape[0], x.shape[1]
    xb = x.reshape(B, S, LRU_BLOCKS, LRU_BLOCK_DIM)
    return jnp.einsum('bsnc,ncd->bsnd', xb, w).reshape(B, S, LRU_WIDTH) + b


def rg_lru(x, wa, ba, wx, bx, lam):
    r = jax.nn.sigmoid(block_diag_linear(x, wa, ba)).astype(jnp.float32)
    i = jax.nn.sigmoid(block_diag_linear(x, wx, bx))
    log_a = -LRU_C * r * jax.nn.softplus(-lam.astype(jnp.float32))
    a = jnp.exp(log_a)
    b = jnp.sqrt(-jnp.expm1(2.0 * log_a)) * (i * x).astype(jnp.float32)

    def combine(c1, c2):
        a1, b1 = c1
        a2, b2 = c2
        return a1 * a2, a2 * b1 + b2

    _, h = lax.associative_scan(combine, (a, b), axis=1)
    return h.astype(x.dtype)


def peer_ffn(x, wq, k1, k2, u, v):
    B, S, D = x.shape
    q = (x @ wq).astype(jnp.float32).reshape(B, S, PEER_HEADS, 2, PEER_HALF)
    s1 = jnp.einsum('bshc,kc->bshk', q[..., 0, :], k1.astype(jnp.float32))
    s2 = jnp.einsum('bshc,kc->bshk', q[..., 1, :], k2.astype(jnp.float32))
    v1, i1 = lax.top_k(s1, PEER_TOPK)
    v2, i2 = lax.top_k(s2, PEER_TOPK)
    cand = (v1[..., :, None] + v2[..., None, :]).reshape(B, S, PEER_HEADS, PEER_TOPK * PEER_TOPK)
    cand_idx = (i1[..., :, None] * PEER_KEYS + i2[..., None, :]).reshape(B, S, PEER_HEADS, PEER_TOPK * PEER_TOPK)
    top_s, pos = lax.top_k(cand, PEER_TOPK)
    idx = jnp.take_along_axis(cand_idx, pos, axis=-1)
    g = jax.nn.softmax(top_s, axis=-1).astype(x.dtype)
    n_chunks = (B * S) // PEER_CHUNK
    xc = x.reshape(n_chunks, PEER_CHUNK, D)
    ic = idx.reshape(n_chunks, PEER_CHUNK, PEER_HEADS, PEER_TOPK)
    gc = g.reshape(n_chunks, PEER_CHUNK, PEER_HEADS, PEER_TOPK)

    def expert_chunk(args):
        xt, it, gt = args
        ue = u[it]
        ve = v[it]
        act = jax.nn.gelu(jnp.einsum('chkd,cd->chk', ue, xt))
        return jnp.einsum('chk,chkd->cd', gt * act, ve)

    y = lax.map(expert_chunk, (xc, ic, gc))
    return y.reshape(B, S, D)


def setup_inputs(seed: int = 0) -> dict:
    key = jax.random.key(seed)
    ks = jax.random.split(key, 32)
    L, D = DEPTH, D_MODEL
    nrm = lambda k, shape, s: jax.random.normal(k, shape, jnp.float32) * s
    a0 = jax.random.uniform(ks[10], (L, LRU_WIDTH), jnp.float32, 0.9, 0.999)
    return {
        "x": nrm(ks[0], (BATCH, SEQ, D), 1.0),
        "p": nrm(ks[1], (L, BATCH, SEQ, PLE_DIM), 1.0),
        "norm_mix_g": 1.0 + nrm(ks[2], (L, D), 0.02),
        "w_in": nrm(ks[3], (L, D, IN_WIDTH), D ** -0.5),
        "attn_sink": nrm(ks[4], (L, N_Q_HEADS), 0.5),
        "conv_w": nrm(ks[5], (L, CONV_WIDTH, LRU_WIDTH), CONV_WIDTH ** -0.5),
        "conv_b": nrm(ks[6], (L, LRU_WIDTH), 0.02),
        "lru_wa": nrm(ks[7], (L, LRU_BLOCKS, LRU_BLOCK_DIM, LRU_BLOCK_DIM), LRU_BLOCK_DIM ** -0.5),
        "lru_ba": nrm(ks[8], (L, LRU_WIDTH), 0.02),
        "lru_wx": nrm(ks[9], (L, LRU_BLOCKS, LRU_BLOCK_DIM, LRU_BLOCK_DIM), LRU_BLOCK_DIM ** -0.5),
        "lru_bx": nrm(ks[11], (L, LRU_WIDTH), 0.02),
        "lru_lambda": jnp.log(a0) - jnp.log1p(-a0),
        "w_up_attn": nrm(ks[12], (L, Q_W, D), Q_W ** -0.5),
        "w_up_lru": nrm(ks[13], (L, LRU_WIDTH, D), LRU_WIDTH ** -0.5),
        "w_o": nrm(ks[14], (L, D, D), D ** -0.5),
        "norm_ffn_g": 1.0 + nrm(ks[15], (L, D), 0.02),
        "peer_wq": nrm(ks[16], (L, D, PEER_HEADS * PEER_QDIM), D ** -0.5),
        "peer_k1": nrm(ks[17], (L, PEER_KEYS, PEER_HALF), PEER_HALF ** -0.5),
        "peer_k2": nrm(ks[18], (L, PEER_KEYS, PEER_HALF), PEER_HALF ** -0.5),
        "peer_u": nrm(ks[19], (L, PEER_N_EXPERTS, D), D ** -0.5),
        "peer_v": nrm(ks[20], (L, PEER_N_EXPERTS, D), PEER_HEADS ** -0.5),
        "norm_ple_g": 1.0 + nrm(ks[21], (L, D), 0.02),
        "ple_w_gate": nrm(ks[22], (L, D, D), D ** -0.5),
        "ple_w_proj": nrm(ks[23], (L, PLE_DIM, D), PLE_DIM ** -0.5),
        "final_g": 1.0 + nrm(ks[24], (D,), 0.02),
    }


def reference(x, p, norm_mix_g, w_in, attn_sink, conv_w, conv_b, lru_wa, lru_ba, lru_wx, lru_bx,
              lru_lambda, w_up_attn, w_up_lru, w_o, norm_ffn_g, peer_wq, peer_k1, peer_k2, peer_u,
              peer_v, norm_ple_g, ple_w_gate, ple_w_proj, final_g):
    B, S, _ = x.shape
    h = x
    for l in range(DEPTH):
        xn = rms_norm(h, norm_mix_g[l])
        proj = xn @ w_in[l]
        q, k, v, lru_x, lru_gate, gate_attn, gate_lru = jnp.split(proj, SPLIT_POINTS, axis=-1)
        attn = sliding_window_attention(q.reshape(B, S, N_Q_HEADS, HEAD_DIM),
                                        k.reshape(B, S, N_KV_HEADS, HEAD_DIM),
                                        v.reshape(B, S, N_KV_HEADS, HEAD_DIM), attn_sink[l])
        lru_in = causal_depthwise_conv(lru_x, conv_w[l], conv_b[l])
        rec = rg_lru(lru_in, lru_wa[l], lru_ba[l], lru_wx[l], lru_bx[l], lru_lambda[l]) * jax.nn.gelu(lru_gate)
        merged = (jax.nn.sigmoid(gate_attn) * (attn @ w_up_attn[l])
                  + jax.nn.sigmoid(gate_lru) * (rec @ w_up_lru[l]))
        h = h + merged @ w_o[l]
        h = h + peer_ffn(rms_norm(h, norm_ffn_g[l]), peer_wq[l], peer_k1[l], peer_k2[l], peer_u[l], peer_v[l])
        gate = jax.nn.sigmoid(rms_norm(h, norm_ple_g[l]) @ ple_w_gate[l])
        h = h + gate * (p[l] @ ple_w_proj[l])
    return rms_norm(h, final_g)
```

```python
from contextlib import ExitStack
from math import prod

import numpy as np
import concourse.bass as bass
import concourse.mybir as mybir
from concourse.bass_utils import run_bass_kernel_spmd

F32 = mybir.dt.float32
BF16 = mybir.dt.bfloat16
U32 = mybir.dt.uint32
AF = mybir.ActivationFunctionType
ALU = mybir.AluOpType
AX = mybir.AxisListType

T = 2048
D = 1024
NT = 16
EPS = 1e-6
ENGS = ("tensor", "vector", "scalar", "gpsimd", "sync")
SEM_CHUNK = 16000


class Buf:
    __slots__ = ("name", "w", "r", "dsem", "dcount")

    def __init__(self, name):
        self.name = name
        self.w = None
        self.r = []
        self.dsem = None
        self.dcount = 0


class Op:
    __slots__ = ("eng", "fn", "deps", "is_dma", "sembuf", "dval", "signal", "sigidx", "sigval")

    def __init__(self, eng, fn, is_dma=False):
        self.eng = eng
        self.fn = fn
        self.deps = []
        self.is_dma = is_dma
        self.sembuf = None
        self.dval = 0
        self.signal = False
        self.sigidx = 0
        self.sigval = 0


class Prog:
    def __init__(self, nc):
        self.nc = nc
        self.ops = {e: [] for e in ENGS}
        self.nbuf = 0
        self.pending = {e: [] for e in ENGS}
        self.dma_since_barrier = []

    def buf(self, name=None):
        self.nbuf += 1
        return Buf(name or f"b{self.nbuf}")

    def bufs(self, n, name="b"):
        return [self.buf(f"{name}{i}") for i in range(n)]

    def _deps(self, op, reads, writes):
        deps = op.deps
        for b in reads:
            if b.w is not None:
                deps.append((b.w, "raw"))
        for b in writes:
            if b.w is not None:
                deps.append((b.w, "waw"))
            for r in b.r:
                deps.append((r, "war"))
        for b in reads:
            b.r.append(op)
        for b in writes:
            b.w = op
            b.r = []
        if self.pending[op.eng]:
            deps.extend(self.pending[op.eng])
            self.pending[op.eng] = []

    def op(self, eng, fn, reads=(), writes=()):
        o = Op(eng, fn)
        self._deps(o, reads, writes)
        self.ops[eng].append(o)
        return o

    def I(self, eng, method, reads=(), writes=(), **kw):
        return self.op(eng, (lambda e, m=method, k=kw: getattr(e, m)(**k)), reads, writes)

    def D(self, queue, out, in_, reads=(), writes=(), sembuf=None, **kw):
        o = Op(queue, (lambda e, o_=out, i_=in_, k=kw: e.dma_start(out=o_, in_=i_, **k)), is_dma=True)
        self._deps(o, reads, writes)
        if sembuf is None:
            sembuf = writes[0]
        o.sembuf = sembuf
        sembuf.dcount += 16
        o.dval = sembuf.dcount
        self.ops[queue].append(o)
        self.dma_since_barrier.append(o)
        return o

    def barrier(self):
        deps = []
        for e in ENGS:
            for o in reversed(self.ops[e]):
                if not o.is_dma:
                    deps.append((o, "bar"))
                    break
        last = {}
        for o in self.dma_since_barrier:
            last[id(o.sembuf)] = o
        deps.extend((o, "bar") for o in last.values())
        self.dma_since_barrier = []
        for e in ENGS:
            self.pending[e] = self.pending[e] + list(deps)

    @staticmethod
    def _needs_wait(o, d, kind):
        if d.is_dma:
            return True
        if d.eng == o.eng and not o.is_dma:
            return d.eng != "tensor"
        return True

    def finish(self, final_waits=()):
        nc = self.nc
        st = ExitStack()
        for e in ENGS:
            for o in self.ops[e]:
                for d, kind in o.deps:
                    if not d.is_dma and self._needs_wait(o, d, kind):
                        d.signal = True
        for o in final_waits:
            if not o.is_dma:
                o.signal = True
        esems = {}
        for e in ENGS:
            n = 0
            for o in self.ops[e]:
                if o.signal and not o.is_dma:
                    o.sigidx = n // SEM_CHUNK
                    o.sigval = n % SEM_CHUNK + 1
                    n += 1
            nsem = max(1, (n + SEM_CHUNK - 1) // SEM_CHUNK)
            esems[e] = [st.enter_context(nc.semaphore(f"s_{e}_{i}")) for i in range(nsem)]
        ndma = 0
        for e in ENGS:
            for o in self.ops[e]:
                if o.is_dma and o.sembuf.dsem is None:
                    o.sembuf.dsem = st.enter_context(nc.semaphore(f"d{ndma}_{o.sembuf.name}"))
                    ndma += 1
        self.n_sems = sum(len(v) for v in esems.values()) + ndma
        prog = self

        def replay(e, eng):
            seen = {}
            for o in prog.ops[e]:
                need = {}
                for d, kind in o.deps:
                    if not prog._needs_wait(o, d, kind):
                        continue
                    if d.is_dma:
                        key = ("d", id(d.sembuf))
                        sem, val = d.sembuf.dsem, d.dval
                    else:
                        key = ("e", d.eng, d.sigidx)
                        sem, val = esems[d.eng][d.sigidx], d.sigval
                    if seen.get(key, 0) >= val:
                        continue
                    if key not in need or need[key][1] < val:
                        need[key] = (sem, val)
                for key, (sem, val) in need.items():
                    eng.wait_ge(sem, val)
                    seen[key] = val
                ins = o.fn(eng)
                if o.is_dma:
                    ins.then_inc(o.sembuf.dsem, 16)
                elif o.signal:
                    ins.then_inc(esems[e][o.sigidx], 1)
            if e == "sync":
                for o in final_waits:
                    if o.is_dma:
                        eng.wait_ge(o.sembuf.dsem, o.dval)
                    else:
                        eng.wait_ge(esems[o.eng][o.sigidx], o.sigval)

        with nc.Block() as block:
            @block.tensor
            def _(eng):
                replay("tensor", eng)

            @block.vector
            def _(eng):
                replay("vector", eng)

            @block.scalar
            def _(eng):
                replay("scalar", eng)

            @block.gpsimd
            def _(eng):
                replay("gpsimd", eng)

            @block.sync
            def _(eng):
                replay("sync", eng)
        st.close()


def mkap(base, offset_elems, dims):
    return bass.AP(base.tensor, offset_elems, [list(d) for d in dims])


V_GMIX, V_CW0, V_CB, V_BA, V_BX, V_LAM, V_GFFN, V_GPLE = 0, 8, 40, 48, 56, 64, 72, 80
NV = 88
C_IDF, C_IOTA, C_MTAB = 0, 128, 256
NCONST = 256 + 2048


def _consts():
    c = np.zeros((128, NCONST), np.float32)
    c[:, C_IDF:C_IDF + 128] = np.eye(128, dtype=np.float32)
    c[:, C_IOTA:C_IOTA + 128] = np.arange(128, dtype=np.float32)[None, :]
    k = np.arange(128)[:, None].astype(np.float64)
    q = np.arange(128)[None, :].astype(np.float64)
    for g in range(2):
        for which in range(2):
            tab = np.zeros((128, 4, 128), np.float64)
            for r in range(4):
                h = g * 4 + r
                slope = 2.0 ** (-8.0 * (h + 1) / 8.0)
                if which == 0:
                    dist = q - k
                    ok = dist >= 0
                else:
                    dist = q + 128 - k
                    ok = dist < 128
                tab[:, r, :] = np.where(ok, np.exp(-slope * dist), 0.0)
            o = C_MTAB + (g * 2 + which) * 512
            c[:, o:o + 512] = tab.reshape(128, 512).astype(np.float32)
    return c


def _col8(v):
    return np.ascontiguousarray(np.asarray(v, np.float32).reshape(8, 128).T)


def _shared_inputs(inp):
    f = lambda k: np.asarray(inp[k], np.float32)
    w_in = f("w_in")[0]
    perm = []
    for j in range(4):
        perm += list(range(j * 64, (j + 1) * 64)) + list(range((4 + j) * 64, (5 + j) * 64))
    w_in_p = np.ascontiguousarray(np.concatenate([w_in[:, perm], w_in[:, 512:]], axis=1))
    vecs = np.zeros((128, NV), np.float32)
    vecs[:, V_GMIX:V_GMIX + 8] = _col8(f("norm_mix_g")[0])
    cw = f("conv_w")[0]
    for tap in range(4):
        vecs[:, V_CW0 + tap * 8:V_CW0 + tap * 8 + 8] = _col8(cw[tap])
    vecs[:, V_CB:V_CB + 8] = _col8(f("conv_b")[0])
    vecs[:, V_BA:V_BA + 8] = _col8(f("lru_ba")[0])
    vecs[:, V_BX:V_BX + 8] = _col8(f("lru_bx")[0])
    vecs[:, V_LAM:V_LAM + 8] = _col8(f("lru_lambda")[0])
    vecs[:, V_GFFN:V_GFFN + 8] = _col8(f("norm_ffn_g")[0])
    vecs[:, V_GPLE:V_GPLE + 8] = _col8(f("norm_ple_g")[0])
    sh = {
        "consts": _consts(),
        "vecs": vecs,
        "sinkb": np.ascontiguousarray(np.broadcast_to(f("attn_sink")[0][None, :], (128, 8))),
        "fgb": np.ascontiguousarray(np.broadcast_to(f("final_g")[None, :], (128, D))),
        "w_in": w_in_p,
        "wa": np.ascontiguousarray(f("lru_wa")[0].transpose(1, 0, 2)),
        "wx": np.ascontiguousarray(f("lru_wx")[0].transpose(1, 0, 2)),
        "w_up_attn": f("w_up_attn")[0],
        "w_up_lru": f("w_up_lru")[0],
        "w_o": f("w_o")[0],
        "wq": f("peer_wq")[0],
        "k1T": np.ascontiguousarray(f("peer_k1")[0].T),
        "k2T": np.ascontiguousarray(f("peer_k2")[0].T),
        "uT": np.ascontiguousarray(f("peer_u")[0].T),
        "pv": f("peer_v")[0],
        "wg": f("ple_w_gate")[0],
        "wp": f("ple_w_proj")[0],
    }
    return sh


IN_SHAPES = {
    "x": [T, D], "p": [T, 256], "consts": [128, NCONST], "vecs": [128, NV], "sinkb": [128, 8], "fgb": [128, D],
    "w_in": [D, 4864], "wa": [128, 8, 128], "wx": [128, 8, 128], "w_up_attn": [512, D], "w_up_lru": [D, D],
    "w_o": [D, D], "wq": [D, 2048], "k1T": [128, 128], "k2T": [128, 128], "uT": [D, 16384], "pv": [16384, D],
    "wg": [D, D], "wp": [256, D],
}
SBUF_BYTES = 208896


class _LazyDram(dict):
    def __init__(self, nc):
        super().__init__()
        self.nc = nc

    def __missing__(self, k):
        v = self.nc.dram_tensor(k, IN_SHAPES[k], F32, kind="ExternalInput").ap()
        self[k] = v
        return v


class Builder:
    def __init__(self, stop_after=None, debug=(), simsafe=False):
        self.simsafe = simsafe
        self.stop_after = stop_after
        self.debug = set(debug)
        nc = self.nc = bass.Bass("TRN2", target_bir_lowering=False)
        self.P = Prog(nc)
        self.dr = _LazyDram(nc)
        self.out = nc.dram_tensor("out", [T, D], F32, kind="ExternalOutput").ap()
        self.S = nc.alloc_sbuf_tensor("S", [128, SBUF_BYTES // 4], F32)
        self.PS = nc.alloc_psum_tensor("PS", [128, 8, 512], F32)
        self.psb = self.P.bufs(8, "ps")
        self.final = []
        self.dbg_out = {}

    def carve(self, off, shape, dt=F32):
        assert off % 4 == 0
        n = prod(shape)
        nb = n * (4 if dt in (F32, U32) else 2)
        assert off + nb <= SBUF_BYTES, (off, nb)
        a = self.S[:, off // 4:(off + nb + 3) // 4]
        if dt != F32:
            a = a.bitcast(dt)
            a = a[:, 0:n]
        if len(shape) == 2:
            a = a.rearrange("p (a b) -> p a b", b=shape[1])
        elif len(shape) == 3:
            a = a.rearrange("p (a b c) -> p a b c", b=shape[1], c=shape[2])
        elif len(shape) == 4:
            a = a.rearrange("p (a b c d) -> p a b c d", b=shape[1], c=shape[2], d=shape[3])
        return a

    def bank(self, b, dt=F32):
        a = self.PS[:, b, :]
        return a if dt == F32 else a.bitcast(dt)

    def banks(self, b0, n):
        return self.PS[:, b0:b0 + n, :].rearrange("p a b -> p (a b)")

    def dump(self, name, ap_, bufs, shape, dt=F32):
        if name not in self.debug:
            return
        d = self.nc.dram_tensor("dbg_" + name, list(shape), dt, kind="ExternalOutput").ap()
        b = self.P.buf("dbg_" + name)
        o = self.P.D("sync", d, ap_, reads=bufs, writes=[b])
        self.final.append(o)
        self.dbg_out[name] = "dbg_" + name

    def rmsnorm_T(self, src, gcol0, dstT, dst_bufs, scr_off, ps_banks):
        P, c = self.P, self
        ssq = c.carve(scr_off, [16])
        std = c.carve(scr_off + 64, [16])
        rstd = c.carve(scr_off + 128, [16])
        junk = c.carve(scr_off + 192, [1024], BF16)
        xs = [c.carve(scr_off + 192 + 2048 + k * 2048, [1024], BF16) for k in range(2)]
        bjunk = P.buf("junk")
        bxs = P.bufs(2, "xs")
        gcol = c.vec[:, gcol0:gcol0 + 8]
        def front(i):
            xa, xb = src(i)
            bst = P.buf("nstat")
            P.I("scalar", "activation", reads=xb, writes=[bjunk, bst], out=junk, in_=xa, func=AF.Square,
                accum_out=ssq[:, i:i + 1])
            P.I("scalar", "activation", reads=[bst], writes=[bst], out=std[:, i:i + 1], in_=ssq[:, i:i + 1],
                func=AF.Sqrt, scale=1.0 / D, bias=c.epsc)
            P.I("vector", "reciprocal", reads=[bst], writes=[bst], out=rstd[:, i:i + 1], in_=std[:, i:i + 1])
            P.I("vector", "tensor_scalar", reads=xb + [bst], writes=[bxs[i % 2]], out=xs[i % 2], in0=xa,
                scalar1=rstd[:, i:i + 1], scalar2=None, op0=ALU.mult)
            pb = ps_banks[i % len(ps_banks)]
            pst = c.bank(pb, BF16).rearrange("p (a b) -> p a b", b=128)
            for k in range(8):
                P.I("tensor", "transpose", reads=[bxs[i % 2], c.bcst], writes=[c.psb[pb]], out=pst[:, k, :],
                    in_=xs[i % 2][:, k * 128:(k + 1) * 128], identity=c.ident_bf)

        def evac(i):
            pb = ps_banks[i % len(ps_banks)]
            pst = c.bank(pb, BF16).rearrange("p (a b) -> p a b", b=128)
            P.I("vector", "tensor_tensor", reads=[c.psb[pb], c.bvec], writes=[dst_bufs[i // 4]],
                out=dstT[:, :, i * 128:(i + 1) * 128], in0=pst, in1=gcol.unsqueeze(2).to_broadcast([128, 8, 128]),
                op=ALU.mult)

        for i in range(NT + 1):
            if i < NT:
                front(i)
            if i >= 1:
                evac(i - 1)

    def proj_fm(self, lhs_of, rhs_of, nk, bank, tb, wbufs, abufs):
        P = self.P
        for k in range(nk):
            P.I("tensor", "matmul", reads=list(wbufs) + list(abufs), writes=[self.psb[bank]], out=self.bank(bank),
                lhsT=lhs_of(k), rhs=rhs_of(k, tb), start=(k == 0), stop=(k == nk - 1))

    def build(self):
        P, c, nc = self.P, self, self.nc
        dr = self.dr
        cst = c.carve(0, [NCONST])
        c.vec = c.carve(9216, [NV])
        c.ident_bf = c.carve(9600, [128], BF16)
        esink = c.carve(9856, [8])
        nsp = c.carve(9888, [8])
        nsp2 = c.carve(9920, [8])
        tmp8 = c.carve(9952, [8])
        c.epsc = c.carve(9984, [1])
        c.onec = c.carve(9988, [1])
        c.bcst, c.bvec = P.buf("cst"), P.buf("vec")
        besink, bnsp = P.buf("esink"), P.buf("nsp")
        c.ident_f = cst[:, C_IDF:C_IDF + 128]
        c.iota = cst[:, C_IOTA:C_IOTA + 128]
        P.D("sync", cst, dr["consts"], writes=[c.bcst])
        P.D("sync", c.vec, dr["vecs"], writes=[c.bvec])
        P.D("sync", esink, dr["sinkb"], writes=[besink])
        P.I("vector", "tensor_copy", reads=[c.bcst], writes=[c.bcst], out=c.ident_bf, in_=c.ident_f)
        P.I("vector", "memset", writes=[c.bcst], ap=c.epsc, constant=EPS)
        P.I("vector", "memset", writes=[c.bcst], ap=c.onec, constant=1.0)
        P.I("scalar", "activation", reads=[besink], writes=[besink], out=esink, in_=esink, func=AF.Exp)
        lam = c.vec[:, V_LAM:V_LAM + 8]
        P.I("scalar", "activation", reads=[c.bvec], writes=[bnsp], out=tmp8, in_=lam, func=AF.Exp, scale=-1.0)
        P.I("vector", "tensor_scalar", reads=[bnsp], writes=[bnsp], out=tmp8, in0=tmp8, scalar1=1.0, scalar2=None,
            op0=ALU.add)
        P.I("scalar", "activation", reads=[bnsp], writes=[bnsp], out=tmp8, in_=tmp8, func=AF.Ln)
        P.I("vector", "tensor_scalar", reads=[bnsp], writes=[bnsp], out=nsp, in0=tmp8, scalar1=-8.0, scalar2=None,
            op0=ALU.mult)
        P.I("vector", "tensor_scalar", reads=[bnsp], writes=[bnsp], out=nsp2, in0=tmp8, scalar1=-16.0, scalar2=None,
            op0=ALU.mult)

        K = 1024
        mergedT = c.carve(12 * K, [8, T], BF16); bmerged = P.bufs(4, "mrg")
        xnT = c.carve(44 * K, [8, T], BF16); bxn = P.bufs(4, "xn")
        attnT = c.carve(76 * K, [4, T], BF16); battn = P.bufs(4, "att")
        recT = c.carve(92 * K, [8, T], BF16); brec = P.bufs(8, "rec")
        TMP = 124 * K

        xt = [c.carve(TMP + k * 4096, [D]) for k in range(4)]
        bxt = P.bufs(4, "xt")

        def src_x(i):
            P.D("sync", xt[i % 4], dr["x"][i * 128:(i + 1) * 128, :], writes=[bxt[i % 4]])
            return xt[i % 4], [bxt[i % 4]]

        c.rmsnorm_T(src_x, V_GMIX, xnT, bxn, TMP + 16384, [6, 7])
        c.dump("xnT", xnT, bxn, [128, 8, T], BF16)
        if c.stop_after == "A1":
            return c.finish()
        P.barrier()

        o = TMP
        wqkv = c.carve(o, [8, 768], BF16); o += 12288; bwqkv = P.buf("wqkv")
        qT = c.carve(o, [4, T], BF16); o += 16384; bq = P.bufs(4, "q")
        kT = c.carve(o, [T], BF16); o += 4096; bk = P.bufs(4, "k")
        vaug = c.carve(o, [16, 2, 65], BF16); o += 4160; bv = P.bufs(4, "v")
        eraw = [c.carve(o + k * 2048, [512]) for k in range(4)]; o += 8192; beraw = P.bufs(4, "eraw")
        ebf = [c.carve(o + k * 1024, [512], BF16) for k in range(4)]; o += 4096; bebf = P.bufs(4, "ebf")
        atok = [c.carve(o + k * 1024, [512], BF16) for k in range(2)]; o += 2048; batok = P.bufs(2, "atok")
        den = [c.carve(o + k * 32, [8]) for k in range(4)]; o += 128; bden = P.bufs(4, "den")
        P.D("gpsimd", wqkv, dr["w_in"][:, 0:768].rearrange("(kc p) n -> p kc n", p=128), writes=[bwqkv])
        P.I("vector", "memset", writes=bv, ap=vaug, constant=1.0)
        xrhs = lambda k, tb: xnT[:, k, tb * 512:(tb + 1) * 512]
        nb = 0
        for j in range(4):
            for tb in range(4):
                b = nb % 4; nb += 1
                c.proj_fm(lambda k: wqkv[:, k, j * 128:(j + 1) * 128], xrhs, 8, b, tb, [bwqkv], [bxn[tb]])
                P.I("scalar", "activation", reads=[c.psb[b]], writes=[bq[tb]], out=qT[:, j, tb * 512:(tb + 1) * 512],
                    in_=c.bank(b), func=AF.Copy, scale=0.125)
        for tb in range(4):
            b = nb % 4; nb += 1
            c.proj_fm(lambda k: wqkv[:, k, 512:640], xrhs, 8, b, tb, [bwqkv], [bxn[tb]])
            P.I("vector", "tensor_copy", reads=[c.psb[b]], writes=[bk[tb]], out=kT[:, tb * 512:(tb + 1) * 512],
                in_=c.bank(b))
        for tb in range(4):
            b = nb % 4; nb += 1
            for ii in range(4):
                i = tb * 4 + ii
                for k in range(8):
                    P.I("tensor", "matmul", reads=[bwqkv, bxn[tb]], writes=[c.psb[b]],
                        out=c.bank(b)[:, ii * 128:(ii + 1) * 128], lhsT=xnT[:, k, i * 128:(i + 1) * 128],
                        rhs=wqkv[:, k, 640:768], start=(k == 0), stop=(k == 7))
            P.I("vector", "tensor_copy", reads=[c.psb[b]], writes=[bv[tb]], out=vaug[:, tb * 4:(tb + 1) * 4, :, 0:64],
                in_=c.bank(b).rearrange("p (a g d) -> p a g d", g=2, d=64))
        mt = lambda g, which: cst[:, C_MTAB + (g * 2 + which) * 512:C_MTAB + (g * 2 + which + 1) * 512]
        units = [(n, g) for n in range(NT) for g in range(2)]
        ust = {}

        def att_front(u):
            n, g = units[u]
            tbn = n // 4
            es = {}
            for which in ([0, 1] if n > 0 else [0]):
                kb = n - which
                sl = (u % 2) * 2 + which
                sb_ = sl
                P.I("tensor", "matmul", reads=[bk[kb // 4], bq[tbn]], writes=[c.psb[sb_]], out=c.bank(sb_),
                    lhsT=kT[g * 64:(g + 1) * 64, kb * 128:(kb + 1) * 128],
                    rhs=qT[g * 64:(g + 1) * 64, :, n * 128:(n + 1) * 128], start=True, stop=True)
                P.I("scalar", "activation", reads=[c.psb[sb_]], writes=[beraw[sl]], out=eraw[sl], in_=c.bank(sb_),
                    func=AF.Exp)
                P.I("vector", "tensor_tensor", reads=[beraw[sl], c.bcst], writes=[bebf[sl]], out=ebf[sl],
                    in0=eraw[sl], in1=mt(g, which), op=ALU.mult)
                es[which] = sl
            ust[u] = es

        def att_back(u):
            n, g = units[u]
            tbn = n // 4
            es = ust.pop(u)
            pvb = 4 + u % 2
            pv = c.bank(pvb)[:, 0:260].rearrange("p (r d) -> p r d", d=65)
            for r in range(4):
                order = [1, 0] if n > 0 else [0]
                for oi, which in enumerate(order):
                    sl = es[which]
                    kb = n - which
                    P.I("tensor", "matmul", reads=[bebf[sl], bv[kb // 4]], writes=[c.psb[pvb]], out=pv[:, r, :],
                        lhsT=ebf[sl][:, r * 128:(r + 1) * 128], rhs=vaug[:, kb, g, :], start=(oi == 0),
                        stop=(oi == len(order) - 1))
            dn = den[u % 4]; bdn = bden[u % 4]
            P.I("vector", "tensor_tensor", reads=[c.psb[pvb], besink], writes=[bdn], out=dn[:, 0:4], in0=pv[:, :, 64],
                in1=esink[:, g * 4:(g + 1) * 4], op=ALU.add)
            P.I("vector", "reciprocal", reads=[bdn], writes=[bdn], out=dn[:, 4:8], in_=dn[:, 0:4])
            P.I("vector", "tensor_tensor", reads=[c.psb[pvb], bdn], writes=[batok[n % 2]],
                out=atok[n % 2][:, g * 256:(g + 1) * 256].rearrange("p (r d) -> p r d", d=64), in0=pv[:, :, 0:64],
                in1=dn[:, 4:8].unsqueeze(2).to_broadcast([128, 4, 64]), op=ALU.mult)
            if g == 1:
                tbank = 6 + n % 2
                pst = c.bank(tbank, BF16).rearrange("p (a b) -> p a b", b=128)
                for k in range(4):
                    P.I("tensor", "transpose", reads=[batok[n % 2], c.bcst], writes=[c.psb[tbank]], out=pst[:, k, :],
                        in_=atok[n % 2][:, k * 128:(k + 1) * 128], identity=c.ident_bf)
                P.I("scalar", "copy", reads=[c.psb[tbank]], writes=[battn[tbn]], out=attnT[:, :, n * 128:(n + 1) * 128],
                    in_=pst[:, 0:4, :])

        for u in range(len(units) + 1):
            if u < len(units):
                att_front(u)
            if u >= 1:
                att_back(u - 1)
        c.dump("attnT", attnT, battn, [128, 4, T], BF16)
        if c.stop_after == "A2":
            return c.finish()
        P.barrier()

        o = TMP
        wl = [c.carve(o + k * 2048, [8, 128], BF16) for k in range(4)]; o += 8192; bwl = P.bufs(4, "wl")
        wawx = c.carve(o, [2, 8, 128], BF16); o += 4096; bwawx = P.buf("wawx")
        y2 = [c.carve(o + k * 8192, [T]) for k in range(2)]; o += 16384; by2 = P.bufs(2, "y")
        ybf2 = [c.carve(o + k * 4096, [T], BF16) for k in range(2)]; o += 8192; bybf2 = P.bufs(2, "ybf")
        rr = c.carve(o, [T]); o += 8192; brr = P.buf("r")
        ii_ = c.carve(o, [T]); o += 8192; bii = P.buf("i")
        aa = c.carve(o, [T]); o += 8192; baa = P.buf("a")
        sq = rr; bsq = brr
        hl = c.carve(o, [T]); o += 8192; bhl = P.buf("hl")
        gl = c.carve(o, [T]); o += 8192; bgl = P.buf("gl")
        P.D("gpsimd", wawx[:, 0], dr["wa"], writes=[bwawx])
        P.D("gpsimd", wawx[:, 1], dr["wx"], writes=[bwawx])
        vcol = lambda base, ch: c.vec[:, base + ch:base + ch + 1]
        q0 = c.banks(0, 4); q0b = c.psb[0:4]
        q1 = c.banks(4, 4); q1b = c.psb[4:8]

        def lru_s1a(ch):
            wlx, wlg = wl[(2 * ch) % 4], wl[(2 * ch + 1) % 4]
            bwlx, bwlg = bwl[(2 * ch) % 4], bwl[(2 * ch + 1) % 4]
            c0 = 768 + ch * 128
            P.D("gpsimd", wlx, dr["w_in"][:, c0:c0 + 128].rearrange("(kc p) n -> p kc n", p=128), writes=[bwlx])
            c1 = 768 + 1024 + ch * 128
            P.D("gpsimd", wlg, dr["w_in"][:, c1:c1 + 128].rearrange("(kc p) n -> p kc n", p=128), writes=[bwlg])
            for tb in range(4):
                c.proj_fm(lambda k: wlx[:, k, :], xrhs, 8, tb, tb, [bwlx], [bxn[tb]])

        def lru_s1b(ch):
            y, by, ybf, bybf = y2[ch % 2], by2[ch % 2], ybf2[ch % 2], bybf2[ch % 2]
            P.I("scalar", "activation", reads=q0b + [c.bvec], writes=[by], out=y, in_=q0, func=AF.Identity,
                scale=vcol(V_CW0 + 24, ch), bias=vcol(V_CB, ch))
            for s in (1, 2, 3):
                P.I("vector", "scalar_tensor_tensor", reads=q0b + [c.bvec, by], writes=[by], out=y[:, s:], in0=q0[:, 0:T - s],
                    scalar=vcol(V_CW0 + (3 - s) * 8, ch), in1=y[:, s:], op0=ALU.mult, op1=ALU.add)
            P.I("scalar", "copy", reads=[by], writes=[bybf], out=ybf, in_=y)
            if ch == 0:
                c.dump("lruin0", y, [by], [128, T])

        lru_s1a(0)
        lru_s1b(0)
        for ch in range(8):
            y, by, ybf, bybf = y2[ch % 2], by2[ch % 2], ybf2[ch % 2], bybf2[ch % 2]
            wlg, bwlg = wl[(2 * ch + 1) % 4], bwl[(2 * ch + 1) % 4]
            for tb in range(4):
                P.I("tensor", "matmul", reads=[bwawx, bybf], writes=[c.psb[4 + tb]], out=c.bank(4 + tb),
                    lhsT=wawx[:, 0, ch, :], rhs=ybf[:, tb * 512:(tb + 1) * 512], start=True, stop=True)
            for tb in range(4):
                P.I("tensor", "matmul", reads=[bwawx, bybf], writes=[c.psb[tb]], out=c.bank(tb),
                    lhsT=wawx[:, 1, ch, :], rhs=ybf[:, tb * 512:(tb + 1) * 512], start=True, stop=True)
            P.I("scalar", "activation", reads=q1b + [c.bvec], writes=[brr], out=rr, in_=q1, func=AF.Sigmoid,
                bias=vcol(V_BA, ch))
            P.I("scalar", "activation", reads=q0b + [c.bvec], writes=[bii], out=ii_, in_=q0, func=AF.Sigmoid,
                bias=vcol(V_BX, ch))
            if ch + 1 < 8:
                lru_s1a(ch + 1)
            for tb in range(4):
                c.proj_fm(lambda k: wlg[:, k, :], xrhs, 8, 4 + tb, tb, [bwlg], [bxn[tb]])
            P.I("scalar", "activation", reads=[brr, bnsp], writes=[baa], out=aa, in_=rr, func=AF.Exp,
                scale=nsp[:, ch:ch + 1])
            P.I("scalar", "activation", reads=[brr, bnsp], writes=[brr], out=rr, in_=rr, func=AF.Exp,
                scale=nsp2[:, ch:ch + 1])
            P.I("vector", "tensor_scalar", reads=[bsq], writes=[bsq], out=sq, in0=sq, scalar1=1.0, scalar2=-1.0,
                op0=ALU.min, op1=ALU.mult)
            P.I("vector", "tensor_tensor", reads=[bii, by], writes=[bii], out=ii_, in0=ii_, in1=y, op=ALU.mult)
            if ch + 1 < 8:
                lru_s1b(ch + 1)
            P.I("scalar", "activation", reads=[bsq], writes=[bsq], out=sq, in_=sq, func=AF.Sqrt, bias=c.onec)
            P.I("vector", "tensor_tensor", reads=[bii, bsq], writes=[bii], out=ii_, in0=ii_, in1=sq, op=ALU.mult)
            P.I("vector", "tensor_tensor_scan", reads=[baa, bii], writes=[bhl], out=hl, data0=aa, data1=ii_, initial=0.0,
                op0=ALU.mult, op1=ALU.add)
            if ch == 0:
                c.dump("hl0", hl, [bhl], [128, T])
            P.I("scalar", "activation", reads=q1b, writes=[bgl], out=gl, in_=q1, func=AF.Gelu_apprx_tanh)
            P.I("vector", "tensor_tensor", reads=[bhl, bgl], writes=[brec[ch]], out=recT[:, ch, :], in0=hl, in1=gl,
                op=ALU.mult)
        c.dump("recT", recT, brec, [128, 8, T], BF16)
        if c.stop_after == "A3":
            return c.finish()
        P.barrier()

        o = TMP
        wgr = [c.carve(o + k * 2048, [8, 128], BF16) for k in range(4)]; o += 8192; bwgr = P.bufs(4, "wgr")
        wua = c.carve(o, [4, D], BF16); o += 8192; bwua = P.buf("wua")
        wul = c.carve(o, [8, D], BF16); o += 16384; bwul = P.buf("wul")
        sg = [c.carve(o + k * 2048, [512]) for k in range(4)]; o += 8192; bsg = P.bufs(4, "sg")
        t12 = [c.carve(o + k * 2048, [512]) for k in range(4)]; o += 8192; bt12 = P.bufs(4, "t12")
        P.D("gpsimd", wua, dr["w_up_attn"].rearrange("(kc p) n -> p kc n", p=128), writes=[bwua])
        P.D("gpsimd", wul, dr["w_up_lru"].rearrange("(kc p) n -> p kc n", p=128), writes=[bwul])
        wo = c.carve(176 * K, [8, D], BF16); bwo = P.buf("wo")
        P.D("gpsimd", wo, dr["w_o"].rearrange("(kc p) n -> p kc n", p=128), writes=[bwo])
        it = 0
        for j in range(8):
            wga, wgl_ = wgr[(2 * j) % 4], wgr[(2 * j + 1) % 4]
            bwga, bwgl = bwgr[(2 * j) % 4], bwgr[(2 * j + 1) % 4]
            c0 = 2816 + j * 128
            P.D("gpsimd", wga, dr["w_in"][:, c0:c0 + 128].rearrange("(kc p) n -> p kc n", p=128), writes=[bwga])
            c1 = 3840 + j * 128
            P.D("gpsimd", wgl_, dr["w_in"][:, c1:c1 + 128].rearrange("(kc p) n -> p kc n", p=128), writes=[bwgl])
            for tb in range(4):
                b0 = (it % 2) * 4; s0 = (it % 2) * 2; it += 1
                c.proj_fm(lambda k: wua[:, k, j * 128:(j + 1) * 128], lambda k, tb_: attnT[:, k, tb_ * 512:(tb_ + 1) * 512],
                          4, b0, tb, [bwua], [battn[tb]])
                c.proj_fm(lambda k: wul[:, k, j * 128:(j + 1) * 128], lambda k, tb_: recT[:, k, tb_ * 512:(tb_ + 1) * 512],
                          8, b0 + 1, tb, [bwul], brec)
                c.proj_fm(lambda k: wga[:, k, :], xrhs, 8, b0 + 2, tb, [bwga], [bxn[tb]])
                c.proj_fm(lambda k: wgl_[:, k, :], xrhs, 8, b0 + 3, tb, [bwgl], [bxn[tb]])
                P.I("scalar", "activation", reads=[c.psb[b0 + 2]], writes=[bsg[s0]], out=sg[s0], in_=c.bank(b0 + 2),
                    func=AF.Sigmoid)
                P.I("scalar", "activation", reads=[c.psb[b0 + 3]], writes=[bsg[s0 + 1]], out=sg[s0 + 1], in_=c.bank(b0 + 3),
                    func=AF.Sigmoid)
                P.I("vector", "tensor_tensor", reads=[bsg[s0], c.psb[b0]], writes=[bt12[s0]], out=t12[s0], in0=sg[s0],
                    in1=c.bank(b0), op=ALU.mult)
                P.I("vector", "tensor_tensor", reads=[bsg[s0 + 1], c.psb[b0 + 1]], writes=[bt12[s0 + 1]], out=t12[s0 + 1],
                    in0=sg[s0 + 1], in1=c.bank(b0 + 1), op=ALU.mult)
                P.I("vector", "tensor_tensor", reads=[bt12[s0], bt12[s0 + 1]], writes=[bmerged[tb]],
                    out=mergedT[:, j, tb * 512:(tb + 1) * 512], in0=t12[s0], in1=t12[s0 + 1], op=ALU.add)
        c.dump("mergedT", mergedT, bmerged, [128, 8, T], BF16)
        if c.stop_after == "A4":
            return c.finish()
        P.barrier()

        h = c.carve(44 * K, [NT, D]); bh = P.bufs(NT, "h")
        c.h, c.bh = h, bh
        xt = [c.carve(124 * K + k * 4096, [D]) for k in range(2)]
        bxt = P.bufs(2, "xt2")
        for i in range(NT):
            P.D("sync", xt[i % 2], dr["x"][i * 128:(i + 1) * 128, :], writes=[bxt[i % 2]])
            b0 = (i % 2) * 2
            for hf in range(2):
                for k in range(8):
                    P.I("tensor", "matmul", reads=[bwo, bmerged[i // 4]], writes=[c.psb[b0 + hf]], out=c.bank(b0 + hf),
                        lhsT=mergedT[:, k, i * 128:(i + 1) * 128], rhs=wo[:, k, hf * 512:(hf + 1) * 512], start=(k == 0),
                        stop=(k == 7))
            P.I("vector", "tensor_tensor", reads=[c.psb[b0], c.psb[b0 + 1], bxt[i % 2]], writes=[bh[i]], out=h[:, i, :],
                in0=c.banks(b0, 2), in1=xt[i % 2], op=ALU.add)
        c.dump("h1", h, bh, [128, NT, D])
        if c.stop_after == "A5":
            return c.finish()
        c.bmerged = bmerged
        self.build_peer()
        if c.stop_after in ("B0", "Bi", "Bi1", "Bii", "Bii1"):
            return c.finish()
        P.barrier()
        self.build_ple()
        return c.finish()

    def build_peer(self):
        P, c, nc = self.P, self, self.nc
        dr = self.dr
        K = 1024
        h, bh = c.h, c.bh
        xn2T = c.carve(12 * K, [8, T], BF16); bxn2 = c.bmerged
        c.xn2T, c.bxn2 = xn2T, bxn2
        c.rmsnorm_T(lambda i: (h[:, i, :], [bh[i]]), V_GFFN, xn2T, bxn2, 157 * K, [6, 7])
        c.dump("xn2T", xn2T, bxn2, [128, 8, T], BF16)
        if c.stop_after == "B0":
            return
        P.barrier()
        kindW = "ExternalOutput" if "Wd" in c.debug else "Internal"
        Wd = nc.dram_tensor("dbg_Wd" if "Wd" in c.debug else "Wd", [128, 16, T, 8], BF16, kind=kindW).ap()
        if "Wd" in c.debug:
            c.dbg_out["Wd"] = "dbg_Wd"
        bWd = P.bufs(NT, "Wd")

        o = 108 * K
        wqr = [c.carve(o + k * 2048, [8, 128], BF16) for k in range(3)]; o += 6144; bwqr = P.bufs(3, "wqr")
        qTb = c.carve(o, [16, 512], BF16); o += 16384; bqTb = P.bufs(16, "qTb")
        kk = c.carve(o, [2, 128], BF16); o += 512; bkk = P.buf("kk")
        s12 = c.carve(o, [2, 8, 128]); o += 8192
        bs1, bs2 = P.bufs(8, "s1_"), P.bufs(8, "s2_")
        cand = c.carve(o - 8192, [8, 112]); bcand = P.bufs(8, "cand")
        v1 = c.carve(o, [8, 16]); o += 512; bv1 = P.bufs(8, "v1_")
        v2 = c.carve(o, [8, 16]); o += 512; bv2 = P.bufs(8, "v2_")
        idx = c.carve(o, [8, 16], U32); o += 512; bidx = P.bufs(8, "idx")
        cc = c.carve(o, [8, 16]); o += 512; bcc = P.bufs(8, "cc")
        tauv = c.carve(o, [8]); o += 32
        zz = c.carve(o, [8]); o += 32
        zinv = c.carve(o, [8]); o += 32
        d16 = c.carve(o, [8, 16]); o += 512
        bsm = P.buf("smalls")
        tok3 = c.carve(o, [3, 128]); o += 1536; btok3 = P.buf("tok3")
        trB = [c.carve(o + k * 512, [128]) for k in range(2)]; o += 1024
        triz = [c.carve(o + k * 512, [2, 128], BF16) for k in range(2)]; o += 1024
        btr = P.bufs(2, "tr")
        iota_bf = c.carve(o, [128], BF16); o += 256
        assert o <= 146 * K, o
        o = 146 * K
        q2rep = [c.carve(o + k * 8192, [32, 128], BF16) for k in range(2)]; o += 16384; bq2rep = [P.bufs(8, f"q2rep{k}_") for k in range(2)]
        oh1 = [c.carve(o + k * 8192, [32, 128], BF16) for k in range(2)]; o += 16384; boh1 = [P.bufs(8, f"oh1{k}_") for k in range(2)]
        fpr = [c.carve(o + k * 2048, [4, 128]) for k in range(3)]; o += 6144; bfpr = P.bufs(3, "fpr")
        rr_ = [c.carve(o + k * 1024, [4, 128], BF16) for k in range(3)]; o += 3072; brr_ = P.bufs(3, "R")
        wsb2 = [c.carve(o + k * 8192, [16, 32, 8], BF16) for k in range(2)]; o += 16384; bwsb2 = P.bufs(2, "wsb")
        assert o <= SBUF_BYTES, o
        P.D("gpsimd", kk[:, 0, :], dr["k1T"], writes=[bkk])
        P.D("gpsimd", kk[:, 1, :], dr["k2T"], writes=[bkk])
        P.I("vector", "tensor_copy", reads=[c.bcst], writes=[c.bcst], out=iota_bf, in_=c.iota)
        ntiles = NT if c.stop_after != "Bi1" else 1
        st = {"nq": 0}

        def tile_prep(i):
            tb, it = i // 4, i % 4
            sl_t = i % 2
            def proj_chunks(ms):
                for m in ms:
                    nq = st["nq"]; st["nq"] += 1
                    w = wqr[nq % 3]; bw = bwqr[nq % 3]; pb = 5 + nq % 2
                    P.D("gpsimd", w, dr["wq"][:, m * 128:(m + 1) * 128].rearrange("(kc p) n -> p kc n", p=128), writes=[bw])
                    yield
                    c.proj_fm(lambda k: w[:, k, :], lambda k, tb_: xn2T[:, k, tb_ * 512:(tb_ + 1) * 512], 8, pb, tb, [bw],
                              [bxn2[tb]])
                    yield
                    P.I("scalar", "copy", reads=[c.psb[pb]], writes=[bqTb[m]], out=qTb[:, m, :], in_=c.bank(pb))
                    yield

            def scores(half):
                for hh in range(8):
                    pb = 5 + hh // 4
                    P.I("tensor", "matmul", reads=[bqTb[2 * hh + half], bkk], writes=[c.psb[pb]],
                        out=c.bank(pb)[:, (hh % 4) * 128:(hh % 4 + 1) * 128], lhsT=qTb[:, 2 * hh + half, it * 128:(it + 1) * 128],
                        rhs=kk[:, half, :], start=True, stop=True)
                    yield
                P.I("scalar", "copy", reads=c.psb[5:7], writes=(bs1 if half == 0 else bs2) + bcand,
                    out=s12[:, half].rearrange("p h k -> p (h k)"), in_=c.banks(5, 2))
                yield

            if it == 0:
                yield from proj_chunks(range(0, 16, 2))
            yield from scores(0)
            for hh in range(8):
                P.I("vector", "max", reads=[bs1[hh]], writes=[bv1[hh]], out=v1[:, hh, 0:8], in_=s12[:, 0, hh, :])
                yield
            for hh in range(8):
                P.I("vector", "max_index", reads=[bs1[hh], bv1[hh]], writes=[bidx[hh]], out=idx[:, hh, 0:8],
                    in_max=v1[:, hh, 0:8], in_values=s12[:, 0, hh, :])
                yield
            for hh in range(8):
                P.I("vector", "match_replace", reads=[bs1[hh], bv1[hh]], writes=[bs1[hh]], out=s12[:, 0, hh, :],
                    in_to_replace=v1[:, hh, 0:8], in_values=s12[:, 0, hh, :], imm_value=-1e30)
                yield
            for hh in range(8):
                P.I("vector", "max", reads=[bs1[hh]], writes=[bv1[hh]], out=v1[:, hh, 8:16], in_=s12[:, 0, hh, :])
                yield
            for hh in range(8):
                P.I("vector", "max_index", reads=[bs1[hh], bv1[hh]], writes=[bidx[hh]], out=idx[:, hh, 8:16],
                    in_max=v1[:, hh, 8:16], in_values=s12[:, 0, hh, :])
                yield
            yield "B"
            if it == 0:
                yield from proj_chunks(range(1, 16, 2))
            yield from scores(1)
            for hh in range(8):
                P.I("vector", "max", reads=[bs2[hh]], writes=[bv2[hh]], out=v2[:, hh, 0:8], in_=s12[:, 1, hh, :])
                yield
            for hh in range(8):
                P.I("vector", "match_replace", reads=[bs2[hh], bv2[hh]], writes=[bs2[hh]], out=s12[:, 1, hh, :],
                    in_to_replace=v2[:, hh, 0:8], in_values=s12[:, 1, hh, :], imm_value=-1e30)
                yield
            for hh in range(8):
                P.I("vector", "max", reads=[bs2[hh]], writes=[bv2[hh]], out=v2[:, hh, 8:16], in_=s12[:, 1, hh, :])
                yield
            P.I("vector", "tensor_tensor", reads=bv1 + bv2, writes=bcand + bs1 + bs2,
                out=cand[:, :, 0:64].rearrange("p h (a b) -> p h a b", b=16),
                in0=v1[:, :, 0:4].unsqueeze(3).to_broadcast([128, 8, 4, 16]),
                in1=v2.unsqueeze(2).to_broadcast([128, 8, 4, 16]), op=ALU.add)
            yield
            P.I("vector", "tensor_tensor", reads=bv1 + bv2, writes=bcand + bs1 + bs2,
                out=cand[:, :, 64:112].rearrange("p h (a b) -> p h a b", b=4),
                in0=v1[:, :, 4:16].unsqueeze(3).to_broadcast([128, 8, 12, 4]),
                in1=v2[:, :, 0:4].unsqueeze(2).to_broadcast([128, 8, 12, 4]), op=ALU.add)
            yield
            for hh in range(8):
                P.I("vector", "max", reads=[bcand[hh]], writes=[bcc[hh]], out=cc[:, hh, 0:8],
                    in_=cand[:, hh, :])
                yield
            for hh in range(8):
                P.I("vector", "match_replace", reads=[bcand[hh], bcc[hh]], writes=[bcand[hh]],
                    out=cand[:, hh, :], in_to_replace=cc[:, hh, 0:8],
                    in_values=cand[:, hh, :], imm_value=-1e30)
                yield
            for hh in range(8):
                P.I("vector", "max", reads=[bcand[hh]], writes=[bcc[hh]], out=cc[:, hh, 8:16],
                    in_=cand[:, hh, :])
                yield
            P.I("vector", "tensor_scalar", reads=bcc, writes=[bsm], out=tauv, in0=cc[:, :, 15], scalar1=-1e-5, scalar2=None,
                op0=ALU.add)
            yield
            P.I("vector", "tensor_tensor", reads=bcc + [bsm], writes=[bsm], out=d16, in0=cc,
                in1=tauv.unsqueeze(2).to_broadcast([128, 8, 16]), op=ALU.subtract)
            yield
            P.I("scalar", "activation", reads=[bsm], writes=[bsm], out=d16, in_=d16, func=AF.Exp)
            yield
            P.I("vector", "reduce_sum", reads=[bsm], writes=[bsm], out=zz, in_=d16, axis=AX.X)
            yield
            P.I("vector", "reciprocal", reads=[bsm], writes=[bsm], out=zinv, in_=zz)
            yield
            P.I("vector", "tensor_tensor", reads=bv1 + [bsm], writes=[btok3],
                out=tok3[:, 0, :].rearrange("p (h a) -> p h a", a=16), in0=v1,
                in1=tauv.unsqueeze(2).to_broadcast([128, 8, 16]), op=ALU.subtract)
            yield
            P.I("vector", "tensor_copy", reads=bidx, writes=[btok3], out=tok3[:, 1, :].rearrange("p (h a) -> p h a", a=16),
                in_=idx)
            yield
            P.I("vector", "tensor_copy", reads=[bsm], writes=[btok3], out=tok3[:, 2, :].rearrange("p (h a) -> p h a", a=16),
                in_=zinv.unsqueeze(2).to_broadcast([128, 8, 16]))
            yield
            for k in range(3):
                P.I("tensor", "transpose", reads=[btok3, c.bcst], writes=[c.psb[7]], out=c.bank(7)[:, k * 128:(k + 1) * 128],
                    in_=tok3[:, k, :], identity=c.ident_f)
                yield
            P.I("scalar", "copy", reads=[c.psb[7]], writes=[btr[sl_t]], out=trB[sl_t], in_=c.bank(7)[:, 0:128])
            yield
            P.I("scalar", "copy", reads=[c.psb[7]], writes=[btr[sl_t]], out=triz[sl_t].rearrange("p k t -> p (k t)"),
                in_=c.bank(7)[:, 128:384])
            yield
            if i == 0:
                c.dump("tok3", tok3, [btok3], [128, 3, 128])

        def sub_prep(i, s_, part=None):
            it = i % 4
            sl = (i * 4 + s_) % 2
            sl_t = i % 2
            for pp in (range(8) if part is None else [part]):
                t0 = s_ * 32 + pp * 4
                lo, hi = pp * 4, pp * 4 + 4
                q2src = qTb[:, :, it * 128 + t0:it * 128 + t0 + 4].rearrange("p (h two) t -> p two t h", two=2)[:, 1]
                q2src = q2src.unsqueeze(3).to_broadcast([128, 4, 8, 16])
                P.I("gpsimd", "tensor_copy", reads=bqTb, writes=[bq2rep[sl][pp]],
                    out=q2rep[sl][:, lo:hi, :].rearrange("p t (h a) -> p t h a", a=16), in_=q2src)
                P.I("vector", "tensor_tensor", reads=[btr[sl_t], c.bcst], writes=[boh1[sl][pp]], out=oh1[sl][:, lo:hi, :],
                    in0=iota_bf.unsqueeze(1).to_broadcast([128, 4, 128]),
                    in1=triz[sl_t][:, 0, t0:t0 + 4].unsqueeze(2).to_broadcast([128, 4, 128]), op=ALU.is_equal)
                for pm in ([pp - 1] if 1 <= pp < 7 else ([6, 7] if pp == 7 else [])):
                    tm = s_ * 32 + pm * 4
                    P.I("vector", "tensor_tensor", reads=[btr[sl_t], boh1[sl][pm]], writes=[boh1[sl][pm]],
                        out=oh1[sl][:, pm * 4:pm * 4 + 4, :], in0=oh1[sl][:, pm * 4:pm * 4 + 4, :],
                        in1=triz[sl_t][:, 1, tm:tm + 4].unsqueeze(2).to_broadcast([128, 4, 128]), op=ALU.mult)

        groups = [(i, s_, gq) for i in range(ntiles) for s_ in range(4) for gq in range(8)]

        def front(G):
            i, s_, gq = groups[G]
            sl = (i * 4 + s_) % 2; sl_t = i % 2; t0 = s_ * 32; tq = gq * 4
            fb = G % 3; ss = G % 3
            for tt in range(4):
                P.I("tensor", "matmul", reads=[bq2rep[sl][gq], bkk], writes=[c.psb[fb]], out=c.bank(fb)[:, tt * 128:(tt + 1) * 128],
                    lhsT=q2rep[sl][:, tq + tt, :], rhs=kk[:, 1, :], start=True, stop=True)
            P.I("vector", "tensor_tensor", reads=[c.psb[fb], btr[sl_t]], writes=[bfpr[ss]], out=fpr[ss],
                in0=c.bank(fb).rearrange("p (t k) -> p t k", k=128),
                in1=trB[sl_t][:, t0 + tq:t0 + tq + 4].unsqueeze(2).to_broadcast([128, 4, 128]), op=ALU.add)
            if c.simsafe:
                P.I("scalar", "activation", reads=[bfpr[ss]], writes=[bfpr[ss]], out=fpr[ss], in_=fpr[ss], func=AF.Relu)
            else:
                P.I("scalar", "activation", reads=[bfpr[ss]], writes=[bfpr[ss]], out=fpr[ss], in_=fpr[ss], func=AF.Prelu,
                    alpha=1.0e5)

        def front2(G):
            ss = G % 3
            P.I("scalar", "activation", reads=[bfpr[ss]], writes=[brr_[ss]], out=rr_[ss], in_=fpr[ss], func=AF.Exp)

        def back(G):
            i, s_, gq = groups[G]
            sl = (i * 4 + s_) % 2; tq = gq * 4
            wb = 3 + G % 2; ss = G % 3
            wsb, bwsb = wsb2[(i * 4 + s_) % 2], bwsb2[(i * 4 + s_) % 2]
            for tt in range(4):
                P.I("tensor", "matmul", reads=[brr_[ss], boh1[sl][gq]], writes=[c.psb[wb]],
                    out=c.bank(wb)[:, tt * 128:(tt + 1) * 128], lhsT=rr_[ss][:, tt, :], rhs=oh1[sl][:, tq + tt, :],
                    start=True, stop=True)
            P.I("scalar", "copy", reads=[c.psb[wb]], writes=[bwsb],
                out=wsb[:, :, tq:tq + 4, :].rearrange("p g t e -> p t g e"),
                in_=c.bank(wb).rearrange("p (t g e) -> p t g e", g=16, e=8))
            if gq == 7:
                P.D("sync", Wd[:, :, i * 128 + s_ * 32:i * 128 + s_ * 32 + 32, :], wsb, reads=[bwsb], writes=[bWd[i]])

        for _ in tile_prep(0):
            pass
        sub_prep(0, 0)
        NG = len(groups)
        gen = None
        hold = False
        for G in range(NG + 2):
            if G < NG:
                i, s_, gq = groups[G]
                if s_ < 3 and gq >= 2:
                    parts = [gq - 2] if gq < 6 else ([4, 5] if gq == 6 else [6, 7])
                    for pp in parts:
                        sub_prep(i, s_ + 1, pp)
                if i + 1 < ntiles:
                    new_tb = ((i + 1) % 4 == 0)
                    if gen is None and s_ == 0 and gq == 3:
                        gen = tile_prep(i + 1)
                        hold = False
                    if gen is not None and gen is not True:
                        if hold and s_ == 3:
                            hold = False
                        last = (s_ == 3 and gq == 3)
                        per_it = 7 if s_ < 3 else 30
                        if not hold:
                            try:
                                for _ in range(10 ** 9 if last else per_it):
                                    if next(gen) == "B" and new_tb and s_ < 3:
                                        hold = True
                                        break
                            except StopIteration:
                                gen = True
                    if s_ == 3 and gq >= 4:
                        for pp in (2 * (gq - 4), 2 * (gq - 4) + 1):
                            sub_prep(i + 1, 0, pp)
                        if gq == 7:
                            gen = None
                front(G)
            if G >= 2:
                back(G - 2)
            if G < NG:
                front2(G)
        if c.stop_after in ("Bi", "Bi1"):
            if "Wd" in c.debug:
                self.final.append(bWd[0].w)
            return
        P.barrier()

        o = 108 * K
        uTr = [c.carve(o + k * 16384, [8, 1024], BF16) for k in range(2)]; o += 32768; buTr = P.bufs(2, "uTr")
        vr = [c.carve(o + k * 16384, [8, 1024], BF16) for k in range(2)]; o += 32768; bvr = P.bufs(2, "vr")
        wc = [c.carve(o + k * 8192, [512, 8], BF16) for k in range(2)]; o += 16384; bwc = P.bufs(2, "wc")
        gg = [c.carve(o + k * 1024, [512], BF16) for k in range(2)]; o += 2048; bgg = P.bufs(2, "gg")
        hh_ = [c.carve(o + k * 8192, [8, 512], BF16) for k in range(1)]; o += 8192; bhh = P.bufs(8, "H")
        assert o <= SBUF_BYTES, o
        ngroups = 16 if c.stop_after != "Bii1" else 1
        na = 0
        ny = 0
        nw = 0
        for g in range(ngroups):
            us, vs = uTr[g % 2], vr[g % 2]
            bus, bvs = buTr[g % 2], bvr[g % 2]
            P.D("gpsimd", us, dr["uT"][:, g * 1024:(g + 1) * 1024].rearrange("(kc p) e -> p kc e", p=128), writes=[bus])
            P.D("gpsimd", vs, dr["pv"][g * 1024:(g + 1) * 1024, :].rearrange("(cc p) d -> p cc d", p=128), writes=[bvs])
            for TB in range(4):
                w_ = wc[nw % 2]; bw_ = bwc[nw % 2]; nw += 1
                P.D("sync", w_, Wd[:, g, TB * 512:(TB + 1) * 512, :], reads=bWd[TB * 4:TB * 4 + 4], writes=[bw_])
                for cch in range(8):
                    ab = na % 2; na += 1
                    for kc in range(8):
                        P.I("tensor", "matmul", reads=[bus, c.bxn2[TB]], writes=[c.psb[ab]], out=c.bank(ab),
                            lhsT=us[:, kc, cch * 128:(cch + 1) * 128], rhs=c.xn2T[:, kc, TB * 512:(TB + 1) * 512],
                            start=(kc == 0), stop=(kc == 7))
                    P.I("scalar", "activation", reads=[c.psb[ab]], writes=[bgg[ab]], out=gg[ab], in_=c.bank(ab),
                        func=AF.Gelu_apprx_tanh)
                    P.I("vector", "tensor_tensor", reads=[bgg[ab], bw_], writes=[bhh[cch]],
                        out=hh_[0][:, cch, :], in0=gg[ab], in1=w_[:, :, cch], op=ALU.mult)
                for hf in range(2):
                    for tt in range(4):
                        yb = 2 + ny % 6; ny += 1
                        for cch in range(8):
                            P.I("tensor", "matmul", reads=[bhh[cch], bvs], writes=[c.psb[yb]], out=c.bank(yb),
                                lhsT=hh_[0][:, cch, tt * 128:(tt + 1) * 128], rhs=vs[:, cch, hf * 512:(hf + 1) * 512],
                                start=(cch == 0), stop=(cch == 7))
                        ti = TB * 4 + tt
                        P.I("vector", "tensor_tensor", reads=[c.psb[yb], bh[ti]], writes=[bh[ti]],
                            out=h[:, ti, hf * 512:(hf + 1) * 512], in0=c.bank(yb), in1=h[:, ti, hf * 512:(hf + 1) * 512],
                            op=ALU.add)
        c.dump("h2", h, bh, [128, NT, D])

    def build_ple(self):
        P, c, nc = self.P, self, self.nc
        dr = self.dr
        K = 1024
        h, bh = c.h, c.bh
        xn3T = c.carve(12 * K, [8, T], BF16); bxn3 = P.bufs(4, "xn3")
        o = 108 * K
        wg = c.carve(o, [8, D], BF16); o += 16384; bwg = P.buf("wg")
        wp = c.carve(o, [2, D], BF16); o += 4096; bwp = P.buf("wp")
        pT = c.carve(o, [2, T], BF16); o += 8192; bpT = P.bufs(NT, "pT")
        fgb = c.carve(o, [D]); o += 4096; bfgb = P.buf("fgb")
        pt = [c.carve(o + k * 1024, [256]) for k in range(2)]; o += 2048; bpt = P.bufs(2, "pt")
        pbf = [c.carve(o + k * 512, [256], BF16) for k in range(2)]; o += 1024; bpbf = P.bufs(2, "pbf")
        sgt = [c.carve(o + k * 4096, [D]) for k in range(2)]; o += 8192; bsgt = P.bufs(2, "sgt")
        ot = [c.carve(o + k * 4096, [D]) for k in range(2)]; o += 8192; bot = P.bufs(2, "ot")
        junk = c.carve(o, [D], BF16); o += 2048; bjunk = P.buf("junk3")
        st3 = c.carve(o, [3, 16]); o += 192
        nscr = o
        P.D("gpsimd", wg, dr["wg"].rearrange("(kc p) n -> p kc n", p=128), writes=[bwg])
        P.D("gpsimd", wp, dr["wp"].rearrange("(kc p) n -> p kc n", p=128), writes=[bwp])
        P.D("sync", fgb, dr["fgb"], writes=[bfgb])
        c.rmsnorm_T(lambda i: (h[:, i, :], [bh[i]]), V_GPLE, xn3T, bxn3, nscr, [6, 7])
        for i in range(NT):
            P.D("sync", pt[i % 2], dr["p"][i * 128:(i + 1) * 128, :], writes=[bpt[i % 2]])
            P.I("vector", "tensor_copy", reads=[bpt[i % 2]], writes=[bpbf[i % 2]], out=pbf[i % 2], in_=pt[i % 2])
            tb_ = i % 2
            pst = c.bank(tb_, BF16).rearrange("p (a b) -> p a b", b=128)
            for k in range(2):
                P.I("tensor", "transpose", reads=[bpbf[i % 2], c.bcst], writes=[c.psb[tb_]], out=pst[:, k, :],
                    in_=pbf[i % 2][:, k * 128:(k + 1) * 128], identity=c.ident_bf)
            P.I("scalar", "copy", reads=[c.psb[tb_]], writes=[bpT[i]], out=pT[:, :, i * 128:(i + 1) * 128], in_=pst[:, 0:2, :])
        bout = P.bufs(2, "outt")

        def stage1(i):
            gb = (i % 2) * 4
            for hf in range(2):
                for kc in range(8):
                    P.I("tensor", "matmul", reads=[bwg, bxn3[i // 4]], writes=[c.psb[gb + hf]], out=c.bank(gb + hf),
                        lhsT=xn3T[:, kc, i * 128:(i + 1) * 128], rhs=wg[:, kc, hf * 512:(hf + 1) * 512], start=(kc == 0),
                        stop=(kc == 7))
            for hf in range(2):
                for kc in range(2):
                    P.I("tensor", "matmul", reads=[bwp, bpT[i]], writes=[c.psb[gb + 2 + hf]], out=c.bank(gb + 2 + hf),
                        lhsT=pT[:, kc, i * 128:(i + 1) * 128], rhs=wp[:, kc, hf * 512:(hf + 1) * 512], start=(kc == 0),
                        stop=(kc == 1))

        def stage2(i):
            gb = (i % 2) * 4
            P.I("scalar", "activation", reads=[c.psb[gb], c.psb[gb + 1]], writes=[bsgt[i % 2]], out=sgt[i % 2],
                in_=c.banks(gb, 2), func=AF.Sigmoid)
            P.I("vector", "tensor_tensor", reads=[bsgt[i % 2], c.psb[gb + 2], c.psb[gb + 3]], writes=[bsgt[i % 2]],
                out=sgt[i % 2], in0=sgt[i % 2], in1=c.banks(gb + 2, 2), op=ALU.mult)
            P.I("vector", "tensor_tensor", reads=[bsgt[i % 2], bh[i]], writes=[bh[i]], out=h[:, i, :], in0=sgt[i % 2],
                in1=h[:, i, :], op=ALU.add)

        def stage3(i):
            bst = P.buf("fstat")
            P.I("scalar", "activation", reads=[bh[i]], writes=[bjunk, bst], out=junk, in_=h[:, i, :], func=AF.Square,
                accum_out=st3[:, 0, i:i + 1])
            P.I("scalar", "activation", reads=[bst], writes=[bst], out=st3[:, 1, i:i + 1], in_=st3[:, 0, i:i + 1],
                func=AF.Sqrt, scale=1.0 / D, bias=c.epsc)
            P.I("vector", "reciprocal", reads=[bst], writes=[bst], out=st3[:, 2, i:i + 1], in_=st3[:, 1, i:i + 1])
            P.I("vector", "scalar_tensor_tensor", reads=[bh[i], bst, bfgb], writes=[bot[i % 2]], out=ot[i % 2], in0=h[:, i, :],
                scalar=st3[:, 2, i:i + 1], in1=fgb, op0=ALU.mult, op1=ALU.mult)
            od = P.D("sync", self.out[i * 128:(i + 1) * 128, :], ot[i % 2], reads=[bot[i % 2]], writes=[bout[i % 2]])
            self.final.append(od)

        for i in range(NT + 1):
            if i < NT:
                stage1(i)
            if i >= 1:
                stage2(i - 1)
        for i in range(NT):
            stage3(i)

    def finish(self):
        self.P.finish(final_waits=self.final)
        return self.nc


def kernel(**inputs):
    nb = 8
    sh = _shared_inputs(inputs)
    x = np.asarray(inputs["x"], np.float32)
    p = np.asarray(inputs["p"], np.float32)
    b = Builder()
    nc = b.build()
    in_maps = []
    for i in range(nb):
        m = dict(sh)
        m["x"] = np.ascontiguousarray(x[i])
        m["p"] = np.ascontiguousarray(p[0, i])
        in_maps.append({k: v for k, v in m.items() if k in b.dr})
    res = run_bass_kernel_spmd(nc, in_maps, core_ids=list(range(nb)))
    return np.stack([np.asarray(res.results[i]["out"], np.float32) for i in range(nb)], axis=0)
```

```python
from contextlib import ExitStack
from math import prod

import numpy as np
import concourse.bass as bass
import concourse.mybir as mybir
from concourse.bass_utils import run_bass_kernel_spmd

F32 = mybir.dt.float32
BF16 = mybir.dt.bfloat16
U32 = mybir.dt.uint32
AF = mybir.ActivationFunctionType
ALU = mybir.AluOpType
AX = mybir.AxisListType

T = 2048
D = 1024
NT = 16
EPS = 1e-6
ENGS = ("tensor", "vector", "scalar", "gpsimd", "sync")
SEM_CHUNK = 16000


class Buf:
    __slots__ = ("name", "w", "r", "dsem", "dcount")

    def __init__(self, name):
        self.name = name
        self.w = None
        self.r = []
        self.dsem = None
        self.dcount = 0


class Op:
    __slots__ = ("eng", "fn", "deps", "is_dma", "sembuf", "dval", "signal", "sigidx", "sigval")

    def __init__(self, eng, fn, is_dma=False):
        self.eng = eng
        self.fn = fn
        self.deps = []
        self.is_dma = is_dma
        self.sembuf = None
        self.dval = 0
        self.signal = False
        self.sigidx = 0
        self.sigval = 0


class Prog:
    def __init__(self, nc):
        self.nc = nc
        self.ops = {e: [] for e in ENGS}
        self.nbuf = 0
        self.pending = {e: [] for e in ENGS}
        self.dma_since_barrier = []

    def buf(self, name=None):
        self.nbuf += 1
        return Buf(name or f"b{self.nbuf}")

    def bufs(self, n, name="b"):
        return [self.buf(f"{name}{i}") for i in range(n)]

    def _deps(self, op, reads, writes):
        deps = op.deps
        for b in reads:
            if b.w is not None:
                deps.append((b.w, "raw"))
        for b in writes:
            if b.w is not None:
                deps.append((b.w, "waw"))
            for r in b.r:
                deps.append((r, "war"))
        for b in reads:
            b.r.append(op)
        for b in writes:
            b.w = op
            b.r = []
        if self.pending[op.eng]:
            deps.extend(self.pending[op.eng])
            self.pending[op.eng] = []

    def op(self, eng, fn, reads=(), writes=()):
        o = Op(eng, fn)
        self._deps(o, reads, writes)
        self.ops[eng].append(o)
        return o

    def I(self, eng, method, reads=(), writes=(), **kw):
        return self.op(eng, (lambda e, m=method, k=kw: getattr(e, m)(**k)), reads, writes)

    def D(self, queue, out, in_, reads=(), writes=(), sembuf=None, **kw):
        o = Op(queue, (lambda e, o_=out, i_=in_, k=kw: e.dma_start(out=o_, in_=i_, **k)), is_dma=True)
        self._deps(o, reads, writes)
        if sembuf is None:
            sembuf = writes[0]
        o.sembuf = sembuf
        sembuf.dcount += 16
        o.dval = sembuf.dcount
        self.ops[queue].append(o)
        self.dma_since_barrier.append(o)
        return o

    def barrier(self):
        deps = []
        for e in ENGS:
            for o in reversed(self.ops[e]):
                if not o.is_dma:
                    deps.append((o, "bar"))
                    break
        last = {}
        for o in self.dma_since_barrier:
            last[id(o.sembuf)] = o
        deps.extend((o, "bar") for o in last.values())
        self.dma_since_barrier = []
        for e in ENGS:
            self.pending[e] = self.pending[e] + list(deps)

    @staticmethod
    def _needs_wait(o, d, kind):
        if d.is_dma:
            return True
        if d.eng == o.eng and not o.is_dma:
            return d.eng != "tensor"
        return True

    def finish(self, final_waits=()):
        nc = self.nc
        st = ExitStack()
        for e in ENGS:
            for o in self.ops[e]:
                for d, kind in o.deps:
                    if not d.is_dma and self._needs_wait(o, d, kind):
                        d.signal = True
        for o in final_waits:
            if not o.is_dma:
                o.signal = True
        esems = {}
        for e in ENGS:
            n = 0
            for o in self.ops[e]:
                if o.signal and not o.is_dma:
                    o.sigidx = n // SEM_CHUNK
                    o.sigval = n % SEM_CHUNK + 1
                    n += 1
            nsem = max(1, (n + SEM_CHUNK - 1) // SEM_CHUNK)
            esems[e] = [st.enter_context(nc.semaphore(f"s_{e}_{i}")) for i in range(nsem)]
        ndma = 0
        for e in ENGS:
            for o in self.ops[e]:
                if o.is_dma and o.sembuf.dsem is None:
                    o.sembuf.dsem = st.enter_context(nc.semaphore(f"d{ndma}_{o.sembuf.name}"))
                    ndma += 1
        self.n_sems = sum(len(v) for v in esems.values()) + ndma
        prog = self

        def replay(e, eng):
            seen = {}
            for o in prog.ops[e]:
                need = {}
                for d, kind in o.deps:
                    if not prog._needs_wait(o, d, kind):
                        continue
                    if d.is_dma:
                        key = ("d", id(d.sembuf))
                        sem, val = d.sembuf.dsem, d.dval
                    else:
                        key = ("e", d.eng, d.sigidx)
                        sem, val = esems[d.eng][d.sigidx], d.sigval
                    if seen.get(key, 0) >= val:
                        continue
                    if key not in need or need[key][1] < val:
                        need[key] = (sem, val)
                for key, (sem, val) in need.items():
                    eng.wait_ge(sem, val)
                    seen[key] = val
                ins = o.fn(eng)
                if o.is_dma:
                    ins.then_inc(o.sembuf.dsem, 16)
                elif o.signal:
                    ins.then_inc(esems[e][o.sigidx], 1)
            if e == "sync":
                for o in final_waits:
                    if o.is_dma:
                        eng.wait_ge(o.sembuf.dsem, o.dval)
                    else:
                        eng.wait_ge(esems[o.eng][o.sigidx], o.sigval)

        with nc.Block() as block:
            @block.tensor
            def _(eng):
                replay("tensor", eng)

            @block.vector
            def _(eng):
                replay("vector", eng)

            @block.scalar
            def _(eng):
                replay("scalar", eng)

            @block.gpsimd
            def _(eng):
                replay("gpsimd", eng)

            @block.sync
            def _(eng):
                replay("sync", eng)
        st.close()


def mkap(base, offset_elems, dims):
    return bass.AP(base.tensor, offset_elems, [list(d) for d in dims])


V_GMIX, V_CW0, V_CB, V_BA, V_BX, V_LAM, V_GFFN, V_GPLE = 0, 8, 40, 48, 56, 64, 72, 80
NV = 88
C_IDF, C_IOTA, C_MTAB = 0, 128, 256
NCONST = 256 + 2048


def _consts():
    c = np.zeros((128, NCONST), np.float32)
    c[:, C_IDF:C_IDF + 128] = np.eye(128, dtype=np.float32)
    c[:, C_IOTA:C_IOTA + 128] = np.arange(128, dtype=np.float32)[None, :]
    k = np.arange(128)[:, None].astype(np.float64)
    q = np.arange(128)[None, :].astype(np.float64)
    for g in range(2):
        for which in range(2):
            tab = np.zeros((128, 4, 128), np.float64)
            for r in range(4):
                h = g * 4 + r
                slope = 2.0 ** (-8.0 * (h + 1) / 8.0)
                if which == 0:
                    dist = q - k
                    ok = dist >= 0
                else:
                    dist = q + 128 - k
                    ok = dist < 128
                tab[:, r, :] = np.where(ok, np.exp(-slope * dist), 0.0)
            o = C_MTAB + (g * 2 + which) * 512
            c[:, o:o + 512] = tab.reshape(128, 512).astype(np.float32)
    return c


def _col8(v):
    return np.ascontiguousarray(np.asarray(v, np.float32).reshape(8, 128).T)


def _shared_inputs(inp):
    f = lambda k: np.asarray(inp[k], np.float32)
    w_in = f("w_in")[0]
    perm = []
    for j in range(4):
        perm += list(range(j * 64, (j + 1) * 64)) + list(range((4 + j) * 64, (5 + j) * 64))
    w_in_p = np.ascontiguousarray(np.concatenate([w_in[:, perm], w_in[:, 512:]], axis=1))
    vecs = np.zeros((128, NV), np.float32)
    vecs[:, V_GMIX:V_GMIX + 8] = _col8(f("norm_mix_g")[0])
    cw = f("conv_w")[0]
    for tap in range(4):
        vecs[:, V_CW0 + tap * 8:V_CW0 + tap * 8 + 8] = _col8(cw[tap])
    vecs[:, V_CB:V_CB + 8] = _col8(f("conv_b")[0])
    vecs[:, V_BA:V_BA + 8] = _col8(f("lru_ba")[0])
    vecs[:, V_BX:V_BX + 8] = _col8(f("lru_bx")[0])
    vecs[:, V_LAM:V_LAM + 8] = _col8(f("lru_lambda")[0])
    vecs[:, V_GFFN:V_GFFN + 8] = _col8(f("norm_ffn_g")[0])
    vecs[:, V_GPLE:V_GPLE + 8] = _col8(f("norm_ple_g")[0])
    sh = {
        "consts": _consts(),
        "vecs": vecs,
        "sinkb": np.ascontiguousarray(np.broadcast_to(f("attn_sink")[0][None, :], (128, 8))),
        "fgb": np.ascontiguousarray(np.broadcast_to(f("final_g")[None, :], (128, D))),
        "w_in": w_in_p,
        "wa": np.ascontiguousarray(f("lru_wa")[0].transpose(1, 0, 2)),
        "wx": np.ascontiguousarray(f("lru_wx")[0].transpose(1, 0, 2)),
        "w_up_attn": f("w_up_attn")[0],
        "w_up_lru": f("w_up_lru")[0],
        "w_o": f("w_o")[0],
        "wq": f("peer_wq")[0],
        "k1T": np.ascontiguousarray(f("peer_k1")[0].T),
        "k2T": np.ascontiguousarray(f("peer_k2")[0].T),
        "uT": np.ascontiguousarray(f("peer_u")[0].T),
        "pv": f("peer_v")[0],
        "wg": f("ple_w_gate")[0],
        "wp": f("ple_w_proj")[0],
    }
    return sh


IN_SHAPES = {
    "x": [T, D], "p": [T, 256], "consts": [128, NCONST], "vecs": [128, NV], "sinkb": [128, 8], "fgb": [128, D],
    "w_in": [D, 4864], "wa": [128, 8, 128], "wx": [128, 8, 128], "w_up_attn": [512, D], "w_up_lru": [D, D],
    "w_o": [D, D], "wq": [D, 2048], "k1T": [128, 128], "k2T": [128, 128], "uT": [D, 16384], "pv": [16384, D],
    "wg": [D, D], "wp": [256, D],
}
SBUF_BYTES = 208896


class _LazyDram(dict):
    def __init__(self, nc):
        super().__init__()
        self.nc = nc

    def __missing__(self, k):
        v = self.nc.dram_tensor(k, IN_SHAPES[k], F32, kind="ExternalInput").ap()
        self[k] = v
        return v


class Builder:
    def __init__(self, stop_after=None, debug=(), simsafe=False):
        self.simsafe = simsafe
        self.stop_after = stop_after
        self.debug = set(debug)
        nc = self.nc = bass.Bass("TRN2", target_bir_lowering=False)
        self.P = Prog(nc)
        self.dr = _LazyDram(nc)
        self.out = nc.dram_tensor("out", [T, D], F32, kind="ExternalOutput").ap()
        self.S = nc.alloc_sbuf_tensor("S", [128, SBUF_BYTES // 4], F32)
        self.PS = nc.alloc_psum_tensor("PS", [128, 8, 512], F32)
        self.psb = self.P.bufs(8, "ps")
        self.final = []
        self.dbg_out = {}

    def carve(self, off, shape, dt=F32):
        assert off % 4 == 0
        n = prod(shape)
        nb = n * (4 if dt in (F32, U32) else 2)
        assert off + nb <= SBUF_BYTES, (off, nb)
        a = self.S[:, off // 4:(off + nb + 3) // 4]
        if dt != F32:
            a = a.bitcast(dt)
            a = a[:, 0:n]
        if len(shape) == 2:
            a = a.rearrange("p (a b) -> p a b", b=shape[1])
        elif len(shape) == 3:
            a = a.rearrange("p (a b c) -> p a b c", b=shape[1], c=shape[2])
        elif len(shape) == 4:
            a = a.rearrange("p (a b c d) -> p a b c d", b=shape[1], c=shape[2], d=shape[3])
        return a

    def bank(self, b, dt=F32):
        a = self.PS[:, b, :]
        return a if dt == F32 else a.bitcast(dt)

    def banks(self, b0, n):
        return self.PS[:, b0:b0 + n, :].rearrange("p a b -> p (a b)")

    def dump(self, name, ap_, bufs, shape, dt=F32):
        if name not in self.debug:
            return
        d = self.nc.dram_tensor("dbg_" + name, list(shape), dt, kind="ExternalOutput").ap()
        b = self.P.buf("dbg_" + name)
        o = self.P.D("sync", d, ap_, reads=bufs, writes=[b])
        self.final.append(o)
        self.dbg_out[name] = "dbg_" + name

    def rmsnorm_T(self, src, gcol0, dstT, dst_bufs, scr_off, ps_banks):
        P, c = self.P, self
        ssq = c.carve(scr_off, [16])
        std = c.carve(scr_off + 64, [16])
        rstd = c.carve(scr_off + 128, [16])
        junk = c.carve(scr_off + 192, [1024], BF16)
        xs = [c.carve(scr_off + 192 + 2048 + k * 2048, [1024], BF16) for k in range(2)]
        bjunk = P.buf("junk")
        bxs = P.bufs(2, "xs")
        gcol = c.vec[:, gcol0:gcol0 + 8]
        def front(i):
            xa, xb = src(i)
            bst = P.buf("nstat")
            P.I("scalar", "activation", reads=xb, writes=[bjunk, bst], out=junk, in_=xa, func=AF.Square,
                accum_out=ssq[:, i:i + 1])
            P.I("scalar", "activation", reads=[bst], writes=[bst], out=std[:, i:i + 1], in_=ssq[:, i:i + 1],
                func=AF.Sqrt, scale=1.0 / D, bias=c.epsc)
            P.I("vector", "reciprocal", reads=[bst], writes=[bst], out=rstd[:, i:i + 1], in_=std[:, i:i + 1])
            P.I("vector", "tensor_scalar", reads=xb + [bst], writes=[bxs[i % 2]], out=xs[i % 2], in0=xa,
                scalar1=rstd[:, i:i + 1], scalar2=None, op0=ALU.mult)
            pb = ps_banks[i % len(ps_banks)]
            pst = c.bank(pb, BF16).rearrange("p (a b) -> p a b", b=128)
            for k in range(8):
                P.I("tensor", "transpose", reads=[bxs[i % 2], c.bcst], writes=[c.psb[pb]], out=pst[:, k, :],
                    in_=xs[i % 2][:, k * 128:(k + 1) * 128], identity=c.ident_bf)

        def evac(i):
            pb = ps_banks[i % len(ps_banks)]
            pst = c.bank(pb, BF16).rearrange("p (a b) -> p a b", b=128)
            P.I("vector", "tensor_tensor", reads=[c.psb[pb], c.bvec], writes=[dst_bufs[i // 4]],
                out=dstT[:, :, i * 128:(i + 1) * 128], in0=pst, in1=gcol.unsqueeze(2).to_broadcast([128, 8, 128]),
                op=ALU.mult)

        for i in range(NT + 1):
            if i < NT:
                front(i)
            if i >= 1:
                evac(i - 1)

    def proj_fm(self, lhs_of, rhs_of, nk, bank, tb, wbufs, abufs):
        P = self.P
        for k in range(nk):
            P.I("tensor", "matmul", reads=list(wbufs) + list(abufs), writes=[self.psb[bank]], out=self.bank(bank),
                lhsT=lhs_of(k), rhs=rhs_of(k, tb), start=(k == 0), stop=(k == nk - 1))

    def build(self):
        P, c, nc = self.P, self, self.nc
        dr = self.dr
        cst = c.carve(0, [NCONST])
        c.vec = c.carve(9216, [NV])
        c.ident_bf = c.carve(9600, [128], BF16)
        esink = c.carve(9856, [8])
        nsp = c.carve(9888, [8])
        nsp2 = c.carve(9920, [8])
        tmp8 = c.carve(9952, [8])
        c.epsc = c.carve(9984, [1])
        c.onec = c.carve(9988, [1])
        c.bcst, c.bvec = P.buf("cst"), P.buf("vec")
        besink, bnsp = P.buf("esink"), P.buf("nsp")
        c.ident_f = cst[:, C_IDF:C_IDF + 128]
        c.iota = cst[:, C_IOTA:C_IOTA + 128]
        P.D("sync", cst, dr["consts"], writes=[c.bcst])
        P.D("sync", c.vec, dr["vecs"], writes=[c.bvec])
        P.D("sync", esink, dr["sinkb"], writes=[besink])
        P.I("vector", "tensor_copy", reads=[c.bcst], writes=[c.bcst], out=c.ident_bf, in_=c.ident_f)
        P.I("vector", "memset", writes=[c.bcst], ap=c.epsc, constant=EPS)
        P.I("vector", "memset", writes=[c.bcst], ap=c.onec, constant=1.0)
        P.I("scalar", "activation", reads=[besink], writes=[besink], out=esink, in_=esink, func=AF.Exp)
        lam = c.vec[:, V_LAM:V_LAM + 8]
        P.I("scalar", "activation", reads=[c.bvec], writes=[bnsp], out=tmp8, in_=lam, func=AF.Exp, scale=-1.0)
        P.I("vector", "tensor_scalar", reads=[bnsp], writes=[bnsp], out=tmp8, in0=tmp8, scalar1=1.0, scalar2=None,
            op0=ALU.add)
        P.I("scalar", "activation", reads=[bnsp], writes=[bnsp], out=tmp8, in_=tmp8, func=AF.Ln)
        P.I("vector", "tensor_scalar", reads=[bnsp], writes=[bnsp], out=nsp, in0=tmp8, scalar1=-8.0, scalar2=None,
            op0=ALU.mult)
        P.I("vector", "tensor_scalar", reads=[bnsp], writes=[bnsp], out=nsp2, in0=tmp8, scalar1=-16.0, scalar2=None,
            op0=ALU.mult)

        K = 1024
        mergedT = c.carve(12 * K, [8, T], BF16); bmerged = P.bufs(4, "mrg")
        xnT = c.carve(44 * K, [8, T], BF16); bxn = P.bufs(4, "xn")
        attnT = c.carve(76 * K, [4, T], BF16); battn = P.bufs(4, "att")
        recT = c.carve(92 * K, [8, T], BF16); brec = P.bufs(8, "rec")
        TMP = 124 * K

        xt = [c.carve(TMP + k * 4096, [D]) for k in range(4)]
        bxt = P.bufs(4, "xt")

        def src_x(i):
            P.D("sync", xt[i % 4], dr["x"][i * 128:(i + 1) * 128, :], writes=[bxt[i % 4]])
            return xt[i % 4], [bxt[i % 4]]

        c.rmsnorm_T(src_x, V_GMIX, xnT, bxn, TMP + 16384, [6, 7])
        c.dump("xnT", xnT, bxn, [128, 8, T], BF16)
        if c.stop_after == "A1":
            return c.finish()
        P.barrier()

        o = TMP
        wqkv = c.carve(o, [8, 768], BF16); o += 12288; bwqkv = P.buf("wqkv")
        qT = c.carve(o, [4, T], BF16); o += 16384; bq = P.bufs(4, "q")
        kT = c.carve(o, [T], BF16); o += 4096; bk = P.bufs(4, "k")
        vaug = c.carve(o, [16, 2, 65], BF16); o += 4160; bv = P.bufs(4, "v")
        eraw = [c.carve(o + k * 2048, [512]) for k in range(4)]; o += 8192; beraw = P.bufs(4, "eraw")
        ebf = [c.carve(o + k * 1024, [512], BF16) for k in range(4)]; o += 4096; bebf = P.bufs(4, "ebf")
        atok = [c.carve(o + k * 1024, [512], BF16) for k in range(2)]; o += 2048; batok = P.bufs(2, "atok")
        den = [c.carve(o + k * 32, [8]) for k in range(4)]; o += 128; bden = P.bufs(4, "den")
        P.D("gpsimd", wqkv, dr["w_in"][:, 0:768].rearrange("(kc p) n -> p kc n", p=128), writes=[bwqkv])
        P.I("vector", "memset", writes=bv, ap=vaug, constant=1.0)
        xrhs = lambda k, tb: xnT[:, k, tb * 512:(tb + 1) * 512]
        nb = 0
        for j in range(4):
            for tb in range(4):
                b = nb % 4; nb += 1
                c.proj_fm(lambda k: wqkv[:, k, j * 128:(j + 1) * 128], xrhs, 8, b, tb, [bwqkv], [bxn[tb]])
                P.I("scalar", "activation", reads=[c.psb[b]], writes=[bq[tb]], out=qT[:, j, tb * 512:(tb + 1) * 512],
                    in_=c.bank(b), func=AF.Copy, scale=0.125)
        for tb in range(4):
            b = nb % 4; nb += 1
            c.proj_fm(lambda k: wqkv[:, k, 512:640], xrhs, 8, b, tb, [bwqkv], [bxn[tb]])
            P.I("vector", "tensor_copy", reads=[c.psb[b]], writes=[bk[tb]], out=kT[:, tb * 512:(tb + 1) * 512],
                in_=c.bank(b))
        for tb in range(4):
            b = nb % 4; nb += 1
            for ii in range(4):
                i = tb * 4 + ii
                for k in range(8):
                    P.I("tensor", "matmul", reads=[bwqkv, bxn[tb]], writes=[c.psb[b]],
                        out=c.bank(b)[:, ii * 128:(ii + 1) * 128], lhsT=xnT[:, k, i * 128:(i + 1) * 128],
                        rhs=wqkv[:, k, 640:768], start=(k == 0), stop=(k == 7))
            P.I("vector", "tensor_copy", reads=[c.psb[b]], writes=[bv[tb]], out=vaug[:, tb * 4:(tb + 1) * 4, :, 0:64],
                in_=c.bank(b).rearrange("p (a g d) -> p a g d", g=2, d=64))
        mt = lambda g, which: cst[:, C_MTAB + (g * 2 + which) * 512:C_MTAB + (g * 2 + which + 1) * 512]
        units = [(n, g) for n in range(NT) for g in range(2)]
        ust = {}

        def att_front(u):
            n, g = units[u]
            tbn = n // 4
            es = {}
            for which in ([0, 1] if n > 0 else [0]):
                kb = n - which
                sl = (u % 2) * 2 + which
                sb_ = sl
                P.I("tensor", "matmul", reads=[bk[kb // 4], bq[tbn]], writes=[c.psb[sb_]], out=c.bank(sb_),
                    lhsT=kT[g * 64:(g + 1) * 64, kb * 128:(kb + 1) * 128],
                    rhs=qT[g * 64:(g + 1) * 64, :, n * 128:(n + 1) * 128], start=True, stop=True)
                P.I("scalar", "activation", reads=[c.psb[sb_]], writes=[beraw[sl]], out=eraw[sl], in_=c.bank(sb_),
                    func=AF.Exp)
                P.I("vector", "tensor_tensor", reads=[beraw[sl], c.bcst], writes=[bebf[sl]], out=ebf[sl],
                    in0=eraw[sl], in1=mt(g, which), op=ALU.mult)
                es[which] = sl
            ust[u] = es

        def att_back(u):
            n, g = units[u]
            tbn = n // 4
            es = ust.pop(u)
            pvb = 4 + u % 2
            pv = c.bank(pvb)[:, 0:260].rearrange("p (r d) -> p r d", d=65)
            for r in range(4):
                order = [1, 0] if n > 0 else [0]
                for oi, which in enumerate(order):
                    sl = es[which]
                    kb = n - which
                    P.I("tensor", "matmul", reads=[bebf[sl], bv[kb // 4]], writes=[c.psb[pvb]], out=pv[:, r, :],
                        lhsT=ebf[sl][:, r * 128:(r + 1) * 128], rhs=vaug[:, kb, g, :], start=(oi == 0),
                        stop=(oi == len(order) - 1))
            dn = den[u % 4]; bdn = bden[u % 4]
            P.I("vector", "tensor_tensor", reads=[c.psb[pvb], besink], writes=[bdn], out=dn[:, 0:4], in0=pv[:, :, 64],
                in1=esink[:, g * 4:(g + 1) * 4], op=ALU.add)
            P.I("vector", "reciprocal", reads=[bdn], writes=[bdn], out=dn[:, 4:8], in_=dn[:, 0:4])
            P.I("vector", "tensor_tensor", reads=[c.psb[pvb], bdn], writes=[batok[n % 2]],
                out=atok[n % 2][:, g * 256:(g + 1) * 256].rearrange("p (r d) -> p r d", d=64), in0=pv[:, :, 0:64],
                in1=dn[:, 4:8].unsqueeze(2).to_broadcast([128, 4, 64]), op=ALU.mult)
            if g == 1:
                tbank = 6 + n % 2
                pst = c.bank(tbank, BF16).rearrange("p (a b) -> p a b", b=128)
                for k in range(4):
                    P.I("tensor", "transpose", reads=[batok[n % 2], c.bcst], writes=[c.psb[tbank]], out=pst[:, k, :],
                        in_=atok[n % 2][:, k * 128:(k + 1) * 128], identity=c.ident_bf)
                P.I("scalar", "copy", reads=[c.psb[tbank]], writes=[battn[tbn]], out=attnT[:, :, n * 128:(n + 1) * 128],
                    in_=pst[:, 0:4, :])

        for u in range(len(units) + 1):
            if u < len(units):
                att_front(u)
            if u >= 1:
                att_back(u - 1)
        c.dump("attnT", attnT, battn, [128, 4, T], BF16)
        if c.stop_after == "A2":
            return c.finish()
        P.barrier()

        o = TMP
        wl = [c.carve(o + k * 2048, [8, 128], BF16) for k in range(4)]; o += 8192; bwl = P.bufs(4, "wl")
        wawx = c.carve(o, [2, 8, 128], BF16); o += 4096; bwawx = P.buf("wawx")
        y2 = [c.carve(o + k * 8192, [T]) for k in range(2)]; o += 16384; by2 = P.bufs(2, "y")
        ybf2 = [c.carve(o + k * 4096, [T], BF16) for k in range(2)]; o += 8192; bybf2 = P.bufs(2, "ybf")
        rr = c.carve(o, [T]); o += 8192; brr = P.buf("r")
        ii_ = c.carve(o, [T]); o += 8192; bii = P.buf("i")
        aa = c.carve(o, [T]); o += 8192; baa = P.buf("a")
        sq = rr; bsq = brr
        hl = c.carve(o, [T]); o += 8192; bhl = P.buf("hl")
        gl = c.carve(o, [T]); o += 8192; bgl = P.buf("gl")
        P.D("gpsimd", wawx[:, 0], dr["wa"], writes=[bwawx])
        P.D("gpsimd", wawx[:, 1], dr["wx"], writes=[bwawx])
        vcol = lambda base, ch: c.vec[:, base + ch:base + ch + 1]
        q0 = c.banks(0, 4); q0b = c.psb[0:4]
        q1 = c.banks(4, 4); q1b = c.psb[4:8]

        def lru_s1a(ch):
            wlx, wlg = wl[(2 * ch) % 4], wl[(2 * ch + 1) % 4]
            bwlx, bwlg = bwl[(2 * ch) % 4], bwl[(2 * ch + 1) % 4]
            c0 = 768 + ch * 128
            P.D("gpsimd", wlx, dr["w_in"][:, c0:c0 + 128].rearrange("(kc p) n -> p kc n", p=128), writes=[bwlx])
            c1 = 768 + 1024 + ch * 128
            P.D("gpsimd", wlg, dr["w_in"][:, c1:c1 + 128].rearrange("(kc p) n -> p kc n", p=128), writes=[bwlg])
            for tb in range(4):
                c.proj_fm(lambda k: wlx[:, k, :], xrhs, 8, tb, tb, [bwlx], [bxn[tb]])

        def lru_s1b(ch):
            y, by, ybf, bybf = y2[ch % 2], by2[ch % 2], ybf2[ch % 2], bybf2[ch % 2]
            P.I("scalar", "activation", reads=q0b + [c.bvec], writes=[by], out=y, in_=q0, func=AF.Identity,
                scale=vcol(V_CW0 + 24, ch), bias=vcol(V_CB, ch))
            for s in (1, 2, 3):
                P.I("vector", "scalar_tensor_tensor", reads=q0b + [c.bvec, by], writes=[by], out=y[:, s:], in0=q0[:, 0:T - s],
                    scalar=vcol(V_CW0 + (3 - s) * 8, ch), in1=y[:, s:], op0=ALU.mult, op1=ALU.add)
            P.I("scalar", "copy", reads=[by], writes=[bybf], out=ybf, in_=y)
            if ch == 0:
                c.dump("lruin0", y, [by], [128, T])

        lru_s1a(0)
        lru_s1b(0)
        for ch in range(8):
            y, by, ybf, bybf = y2[ch % 2], by2[ch % 2], ybf2[ch % 2], bybf2[ch % 2]
            wlg, bwlg = wl[(2 * ch + 1) % 4], bwl[(2 * ch + 1) % 4]
            for tb in range(4):
                P.I("tensor", "matmul", reads=[bwawx, bybf], writes=[c.psb[4 + tb]], out=c.bank(4 + tb),
                    lhsT=wawx[:, 0, ch, :], rhs=ybf[:, tb * 512:(tb + 1) * 512], start=True, stop=True)
            for tb in range(4):
                P.I("tensor", "matmul", reads=[bwawx, bybf], writes=[c.psb[tb]], out=c.bank(tb),
                    lhsT=wawx[:, 1, ch, :], rhs=ybf[:, tb * 512:(tb + 1) * 512], start=True, stop=True)
            P.I("scalar", "activation", reads=q1b + [c.bvec], writes=[brr], out=rr, in_=q1, func=AF.Sigmoid,
                bias=vcol(V_BA, ch))
            P.I("scalar", "activation", reads=q0b + [c.bvec], writes=[bii], out=ii_, in_=q0, func=AF.Sigmoid,
                bias=vcol(V_BX, ch))
            if ch + 1 < 8:
                lru_s1a(ch + 1)
            for tb in range(4):
                c.proj_fm(lambda k: wlg[:, k, :], xrhs, 8, 4 + tb, tb, [bwlg], [bxn[tb]])
            P.I("scalar", "activation", reads=[brr, bnsp], writes=[baa], out=aa, in_=rr, func=AF.Exp,
                scale=nsp[:, ch:ch + 1])
            P.I("scalar", "activation", reads=[brr, bnsp], writes=[brr], out=rr, in_=rr, func=AF.Exp,
                scale=nsp2[:, ch:ch + 1])
            P.I("vector", "tensor_scalar", reads=[bsq], writes=[bsq], out=sq, in0=sq, scalar1=1.0, scalar2=-1.0,
                op0=ALU.min, op1=ALU.mult)
            P.I("vector", "tensor_tensor", reads=[bii, by], writes=[bii], out=ii_, in0=ii_, in1=y, op=ALU.mult)
            if ch + 1 < 8:
                lru_s1b(ch + 1)
            P.I("scalar", "activation", reads=[bsq], writes=[bsq], out=sq, in_=sq, func=AF.Sqrt, bias=c.onec)
            P.I("vector", "tensor_tensor", reads=[bii, bsq], writes=[bii], out=ii_, in0=ii_, in1=sq, op=ALU.mult)
            P.I("vector", "tensor_tensor_scan", reads=[baa, bii], writes=[bhl], out=hl, data0=aa, data1=ii_, initial=0.0,
                op0=ALU.mult, op1=ALU.add)
            if ch == 0:
                c.dump("hl0", hl, [bhl], [128, T])
            P.I("scalar", "activation", reads=q1b, writes=[bgl], out=gl, in_=q1, func=AF.Gelu_apprx_tanh)
            P.I("vector", "tensor_tensor", reads=[bhl, bgl], writes=[brec[ch]], out=recT[:, ch, :], in0=hl, in1=gl,
                op=ALU.mult)
        c.dump("recT", recT, brec, [128, 8, T], BF16)
        if c.stop_after == "A3":
            return c.finish()
        P.barrier()

        o = TMP
        wgr = [c.carve(o + k * 2048, [8, 128], BF16) for k in range(4)]; o += 8192; bwgr = P.bufs(4, "wgr")
        wua = c.carve(o, [4, D], BF16); o += 8192; bwua = P.buf("wua")
        wul = c.carve(o, [8, D], BF16); o += 16384; bwul = P.buf("wul")
        sg = [c.carve(o + k * 2048, [512]) for k in range(4)]; o += 8192; bsg = P.bufs(4, "sg")
        t12 = [c.carve(o + k * 2048, [512]) for k in range(4)]; o += 8192; bt12 = P.bufs(4, "t12")
        P.D("gpsimd", wua, dr["w_up_attn"].rearrange("(kc p) n -> p kc n", p=128), writes=[bwua])
        P.D("gpsimd", wul, dr["w_up_lru"].rearrange("(kc p) n -> p kc n", p=128), writes=[bwul])
        wo = c.carve(176 * K, [8, D], BF16); bwo = P.buf("wo")
        P.D("gpsimd", wo, dr["w_o"].rearrange("(kc p) n -> p kc n", p=128), writes=[bwo])
        it = 0
        for j in range(8):
            wga, wgl_ = wgr[(2 * j) % 4], wgr[(2 * j + 1) % 4]
            bwga, bwgl = bwgr[(2 * j) % 4], bwgr[(2 * j + 1) % 4]
            c0 = 2816 + j * 128
            P.D("gpsimd", wga, dr["w_in"][:, c0:c0 + 128].rearrange("(kc p) n -> p kc n", p=128), writes=[bwga])
            c1 = 3840 + j * 128
            P.D("gpsimd", wgl_, dr["w_in"][:, c1:c1 + 128].rearrange("(kc p) n -> p kc n", p=128), writes=[bwgl])
            for tb in range(4):
                b0 = (it % 2) * 4; s0 = (it % 2) * 2; it += 1
                c.proj_fm(lambda k: wua[:, k, j * 128:(j + 1) * 128], lambda k, tb_: attnT[:, k, tb_ * 512:(tb_ + 1) * 512],
                          4, b0, tb, [bwua], [battn[tb]])
                c.proj_fm(lambda k: wul[:, k, j * 128:(j + 1) * 128], lambda k, tb_: recT[:, k, tb_ * 512:(tb_ + 1) * 512],
                          8, b0 + 1, tb, [bwul], brec)
                c.proj_fm(lambda k: wga[:, k, :], xrhs, 8, b0 + 2, tb, [bwga], [bxn[tb]])
                c.proj_fm(lambda k: wgl_[:, k, :], xrhs, 8, b0 + 3, tb, [bwgl], [bxn[tb]])
                P.I("scalar", "activation", reads=[c.psb[b0 + 2]], writes=[bsg[s0]], out=sg[s0], in_=c.bank(b0 + 2),
                    func=AF.Sigmoid)
                P.I("scalar", "activation", reads=[c.psb[b0 + 3]], writes=[bsg[s0 + 1]], out=sg[s0 + 1], in_=c.bank(b0 + 3),
                    func=AF.Sigmoid)
                P.I("vector", "tensor_tensor", reads=[bsg[s0], c.psb[b0]], writes=[bt12[s0]], out=t12[s0], in0=sg[s0],
                    in1=c.bank(b0), op=ALU.mult)
                P.I("vector", "tensor_tensor", reads=[bsg[s0 + 1], c.psb[b0 + 1]], writes=[bt12[s0 + 1]], out=t12[s0 + 1],
                    in0=sg[s0 + 1], in1=c.bank(b0 + 1), op=ALU.mult)
                P.I("vector", "tensor_tensor", reads=[bt12[s0], bt12[s0 + 1]], writes=[bmerged[tb]],
                    out=mergedT[:, j, tb * 512:(tb + 1) * 512], in0=t12[s0], in1=t12[s0 + 1], op=ALU.add)
        c.dump("mergedT", mergedT, bmerged, [128, 8, T], BF16)
        if c.stop_after == "A4":
            return c.finish()
        P.barrier()

        h = c.carve(44 * K, [NT, D]); bh = P.bufs(NT, "h")
        c.h, c.bh = h, bh
        xt = [c.carve(124 * K + k * 4096, [D]) for k in range(2)]
        bxt = P.bufs(2, "xt2")
        for i in range(NT):
            P.D("sync", xt[i % 2], dr["x"][i * 128:(i + 1) * 128, :], writes=[bxt[i % 2]])
            b0 = (i % 2) * 2
            for hf in range(2):
                for k in range(8):
                    P.I("tensor", "matmul", reads=[bwo, bmerged[i // 4]], writes=[c.psb[b0 + hf]], out=c.bank(b0 + hf),
                        lhsT=mergedT[:, k, i * 128:(i + 1) * 128], rhs=wo[:, k, hf * 512:(hf + 1) * 512], start=(k == 0),
                        stop=(k == 7))
            P.I("vector", "tensor_tensor", reads=[c.psb[b0], c.psb[b0 + 1], bxt[i % 2]], writes=[bh[i]], out=h[:, i, :],
                in0=c.banks(b0, 2), in1=xt[i % 2], op=ALU.add)
        c.dump("h1", h, bh, [128, NT, D])
        if c.stop_after == "A5":
            return c.finish()
        c.bmerged = bmerged
        self.build_peer()
        if c.stop_after in ("B0", "Bi", "Bi1", "Bii", "Bii1"):
            return c.finish()
        P.barrier()
        self.build_ple()
        return c.finish()

    def build_peer(self):
        P, c, nc = self.P, self, self.nc
        dr = self.dr
        K = 1024
        h, bh = c.h, c.bh
        xn2T = c.carve(12 * K, [8, T], BF16); bxn2 = c.bmerged
        c.xn2T, c.bxn2 = xn2T, bxn2
        c.rmsnorm_T(lambda i: (h[:, i, :], [bh[i]]), V_GFFN, xn2T, bxn2, 157 * K, [6, 7])
        c.dump("xn2T", xn2T, bxn2, [128, 8, T], BF16)
        if c.stop_after == "B0":
            return
        P.barrier()
        kindW = "ExternalOutput" if "Wd" in c.debug else "Internal"
        Wd = nc.dram_tensor("dbg_Wd" if "Wd" in c.debug else "Wd", [128, 16, T, 8], BF16, kind=kindW).ap()
        if "Wd" in c.debug:
            c.dbg_out["Wd"] = "dbg_Wd"
        bWd = P.bufs(NT, "Wd")

        o = 108 * K
        wqr = [c.carve(o + k * 2048, [8, 128], BF16) for k in range(3)]; o += 6144; bwqr = P.bufs(3, "wqr")
        qTb = c.carve(o, [16, 512], BF16); o += 16384; bqTb = P.bufs(16, "qTb")
        kk = c.carve(o, [2, 128], BF16); o += 512; bkk = P.buf("kk")
        s12 = c.carve(o, [2, 8, 128]); o += 8192
        bs1, bs2 = P.bufs(8, "s1_"), P.bufs(8, "s2_")
        cand = c.carve(o - 8192, [8, 112]); bcand = P.bufs(8, "cand")
        v1 = c.carve(o, [8, 16]); o += 512; bv1 = P.bufs(8, "v1_")
        v2 = c.carve(o, [8, 16]); o += 512; bv2 = P.bufs(8, "v2_")
        idx = c.carve(o, [8, 16], U32); o += 512; bidx = P.bufs(8, "idx")
        cc = c.carve(o, [8, 16]); o += 512; bcc = P.bufs(8, "cc")
        tauv = c.carve(o, [8]); o += 32
        zz = c.carve(o, [8]); o += 32
        zinv = c.carve(o, [8]); o += 32
        d16 = c.carve(o, [8, 16]); o += 512
        bsm = P.buf("smalls")
        tok3 = c.carve(o, [3, 128]); o += 1536; btok3 = P.buf("tok3")
        trB = [c.carve(o + k * 512, [128]) for k in range(2)]; o += 1024
        triz = [c.carve(o + k * 512, [2, 128], BF16) for k in range(2)]; o += 1024
        btr = P.bufs(2, "tr")
        iota_bf = c.carve(o, [128], BF16); o += 256
        assert o <= 146 * K, o
        o = 146 * K
        q2rep = [c.carve(o + k * 8192, [32, 128], BF16) for k in range(2)]; o += 16384; bq2rep = [P.bufs(8, f"q2rep{k}_") for k in range(2)]
        oh1 = [c.carve(o + k * 8192, [32, 128], BF16) for k in range(2)]; o += 16384; boh1 = [P.bufs(8, f"oh1{k}_") for k in range(2)]
        fpr = [c.carve(o + k * 2048, [4, 128]) for k in range(3)]; o += 6144; bfpr = P.bufs(3, "fpr")
        rr_ = [c.carve(o + k * 1024, [4, 128], BF16) for k in range(3)]; o += 3072; brr_ = P.bufs(3, "R")
        wsb2 = [c.carve(o + k * 8192, [16, 32, 8], BF16) for k in range(2)]; o += 16384; bwsb2 = P.bufs(2, "wsb")
        assert o <= SBUF_BYTES, o
        P.D("gpsimd", kk[:, 0, :], dr["k1T"], writes=[bkk])
        P.D("gpsimd", kk[:, 1, :], dr["k2T"], writes=[bkk])
        P.I("vector", "tensor_copy", reads=[c.bcst], writes=[c.bcst], out=iota_bf, in_=c.iota)
        ntiles = NT if c.stop_after != "Bi1" else 1
        st = {"nq": 0}

        def tile_prep(i):
            tb, it = i // 4, i % 4
            sl_t = i % 2
            def proj_chunks(ms):
                for m in ms:
                    nq = st["nq"]; st["nq"] += 1
                    w = wqr[nq % 3]; bw = bwqr[nq % 3]; pb = 5 + nq % 2
                    P.D("gpsimd", w, dr["wq"][:, m * 128:(m + 1) * 128].rearrange("(kc p) n -> p kc n", p=128), writes=[bw])
                    yield
                    c.proj_fm(lambda k: w[:, k, :], lambda k, tb_: xn2T[:, k, tb_ * 512:(tb_ + 1) * 512], 8, pb, tb, [bw],
                              [bxn2[tb]])
                    yield
                    P.I("scalar", "copy", reads=[c.psb[pb]], writes=[bqTb[m]], out=qTb[:, m, :], in_=c.bank(pb))
                    yield

            def scores(half):
                for hh in range(8):
                    pb = 5 + hh // 4
                    P.I("tensor", "matmul", reads=[bqTb[2 * hh + half], bkk], writes=[c.psb[pb]],
                        out=c.bank(pb)[:, (hh % 4) * 128:(hh % 4 + 1) * 128], lhsT=qTb[:, 2 * hh + half, it * 128:(it + 1) * 128],
                        rhs=kk[:, half, :], start=True, stop=True)
                    yield
                P.I("scalar", "copy", reads=c.psb[5:7], writes=(bs1 if half == 0 else bs2) + bcand,
                    out=s12[:, half].rearrange("p h k -> p (h k)"), in_=c.banks(5, 2))
                yield

            if it == 0:
                yield from proj_chunks(range(0, 16, 2))
            yield from scores(0)
            for hh in range(8):
                P.I("vector", "max", reads=[bs1[hh]], writes=[bv1[hh]], out=v1[:, hh, 0:8], in_=s12[:, 0, hh, :])
                yield
            for hh in range(8):
                P.I("vector", "max_index", reads=[bs1[hh], bv1[hh]], writes=[bidx[hh]], out=idx[:, hh, 0:8],
                    in_max=v1[:, hh, 0:8], in_values=s12[:, 0, hh, :])
                yield
            for hh in range(8):
                P.I("vector", "match_replace", reads=[bs1[hh], bv1[hh]], writes=[bs1[hh]], out=s12[:, 0, hh, :],
                    in_to_replace=v1[:, hh, 0:8], in_values=s12[:, 0, hh, :], imm_value=-1e30)
                yield
            for hh in range(8):
                P.I("vector", "max", reads=[bs1[hh]], writes=[bv1[hh]], out=v1[:, hh, 8:16], in_=s12[:, 0, hh, :])
                yield
            for hh in range(8):
                P.I("vector", "max_index", reads=[bs1[hh], bv1[hh]], writes=[bidx[hh]], out=idx[:, hh, 8:16],
                    in_max=v1[:, hh, 8:16], in_values=s12[:, 0, hh, :])
                yield
            yield "B"
            if it == 0:
                yield from proj_chunks(range(1, 16, 2))
            yield from scores(1)
            for hh in range(8):
                P.I("vector", "max", reads=[bs2[hh]], writes=[bv2[hh]], out=v2[:, hh, 0:8], in_=s12[:, 1, hh, :])
                yield
            for hh in range(8):
                P.I("vector", "match_replace", reads=[bs2[hh], bv2[hh]], writes=[bs2[hh]], out=s12[:, 1, hh, :],
                    in_to_replace=v2[:, hh, 0:8], in_values=s12[:, 1, hh, :], imm_value=-1e30)
                yield
            for hh in range(8):
                P.I("vector", "max", reads=[bs2[hh]], writes=[bv2[hh]], out=v2[:, hh, 8:16], in_=s12[:, 1, hh, :])
                yield
            P.I("vector", "tensor_tensor", reads=bv1 + bv2, writes=bcand + bs1 + bs2,
                out=cand[:, :, 0:64].rearrange("p h (a b) -> p h a b", b=16),
                in0=v1[:, :, 0:4].unsqueeze(3).to_broadcast([128, 8, 4, 16]),
                in1=v2.unsqueeze(2).to_broadcast([128, 8, 4, 16]), op=ALU.add)
            yield
            P.I("vector", "tensor_tensor", reads=bv1 + bv2, writes=bcand + bs1 + bs2,
                out=cand[:, :, 64:112].rearrange("p h (a b) -> p h a b", b=4),
                in0=v1[:, :, 4:16].unsqueeze(3).to_broadcast([128, 8, 12, 4]),
                in1=v2[:, :, 0:4].unsqueeze(2).to_broadcast([128, 8, 12, 4]), op=ALU.add)
            yield
            for hh in range(8):
                P.I("vector", "max", reads=[bcand[hh]], writes=[bcc[hh]], out=cc[:, hh, 0:8],
                    in_=cand[:, hh, :])
                yield
            for hh in range(8):
                P.I("vector", "match_replace", reads=[bcand[hh], bcc[hh]], writes=[bcand[hh]],
                    out=cand[:, hh, :], in_to_replace=cc[:, hh, 0:8],
                    in_values=cand[:, hh, :], imm_value=-1e30)
                yield
            for hh in range(8):
                P.I("vector", "max", reads=[bcand[hh]], writes=[bcc[hh]], out=cc[:, hh, 8:16],
                    in_=cand[:, hh, :])
                yield
            P.I("vector", "tensor_scalar", reads=bcc, writes=[bsm], out=tauv, in0=cc[:, :, 15], scalar1=-1e-5, scalar2=None,
                op0=ALU.add)
            yield
            P.I("vector", "tensor_tensor", reads=bcc + [bsm], writes=[bsm], out=d16, in0=cc,
                in1=tauv.unsqueeze(2).to_broadcast([128, 8, 16]), op=ALU.subtract)
            yield
            P.I("scalar", "activation", reads=[bsm], writes=[bsm], out=d16, in_=d16, func=AF.Exp)
            yield
            P.I("vector", "reduce_sum", reads=[bsm], writes=[bsm], out=zz, in_=d16, axis=AX.X)
            yield
            P.I("vector", "reciprocal", reads=[bsm], writes=[bsm], out=zinv, in_=zz)
            yield
            P.I("vector", "tensor_tensor", reads=bv1 + [bsm], writes=[btok3],
                out=tok3[:, 0, :].rearrange("p (h a) -> p h a", a=16), in0=v1,
                in1=tauv.unsqueeze(2).to_broadcast([128, 8, 16]), op=ALU.subtract)
            yield
            P.I("vector", "tensor_copy", reads=bidx, writes=[btok3], out=tok3[:, 1, :].rearrange("p (h a) -> p h a", a=16),
                in_=idx)
            yield
            P.I("vector", "tensor_copy", reads=[bsm], writes=[btok3], out=tok3[:, 2, :].rearrange("p (h a) -> p h a", a=16),
                in_=zinv.unsqueeze(2).to_broadcast([128, 8, 16]))
            yield
            for k in range(3):
                P.I("tensor", "transpose", reads=[btok3, c.bcst], writes=[c.psb[7]], out=c.bank(7)[:, k * 128:(k + 1) * 128],
                    in_=tok3[:, k, :], identity=c.ident_f)
                yield
            P.I("scalar", "copy", reads=[c.psb[7]], writes=[btr[sl_t]], out=trB[sl_t], in_=c.bank(7)[:, 0:128])
            yield
            P.I("scalar", "copy", reads=[c.psb[7]], writes=[btr[sl_t]], out=triz[sl_t].rearrange("p k t -> p (k t)"),
                in_=c.bank(7)[:, 128:384])
            yield
            if i == 0:
                c.dump("tok3", tok3, [btok3], [128, 3, 128])

        def sub_prep(i, s_, part=None):
            it = i % 4
            sl = (i * 4 + s_) % 2
            sl_t = i % 2
            for pp in (range(8) if part is None else [part]):
                t0 = s_ * 32 + pp * 4
                lo, hi = pp * 4, pp * 4 + 4
                q2src = qTb[:, :, it * 128 + t0:it * 128 + t0 + 4].rearrange("p (h two) t -> p two t h", two=2)[:, 1]
                q2src = q2src.unsqueeze(3).to_broadcast([128, 4, 8, 16])
                P.I("scalar", "copy", reads=bqTb, writes=[bq2rep[sl][pp]],
                    out=q2rep[sl][:, lo:hi, :].rearrange("p t (h a) -> p t h a", a=16), in_=q2src)
                P.I("vector", "tensor_tensor", reads=[btr[sl_t], c.bcst], writes=[boh1[sl][pp]], out=oh1[sl][:, lo:hi, :],
                    in0=iota_bf.unsqueeze(1).to_broadcast([128, 4, 128]),
                    in1=triz[sl_t][:, 0, t0:t0 + 4].unsqueeze(2).to_broadcast([128, 4, 128]), op=ALU.is_equal)
                for pm in ([pp - 1] if 1 <= pp < 7 else ([6, 7] if pp == 7 else [])):
                    tm = s_ * 32 + pm * 4
                    P.I("vector", "tensor_tensor", reads=[btr[sl_t], boh1[sl][pm]], writes=[boh1[sl][pm]],
                        out=oh1[sl][:, pm * 4:pm * 4 + 4, :], in0=oh1[sl][:, pm * 4:pm * 4 + 4, :],
                        in1=triz[sl_t][:, 1, tm:tm + 4].unsqueeze(2).to_broadcast([128, 4, 128]), op=ALU.mult)

        groups = [(i, s_, gq) for i in range(ntiles) for s_ in range(4) for gq in range(8)]

        def front(G):
            i, s_, gq = groups[G]
            sl = (i * 4 + s_) % 2; sl_t = i % 2; t0 = s_ * 32; tq = gq * 4
            fb = G % 3; ss = G % 3
            for tt in range(4):
                P.I("tensor", "matmul", reads=[bq2rep[sl][gq], bkk], writes=[c.psb[fb]], out=c.bank(fb)[:, tt * 128:(tt + 1) * 128],
                    lhsT=q2rep[sl][:, tq + tt, :], rhs=kk[:, 1, :], start=True, stop=True)
            P.I("vector", "tensor_tensor", reads=[c.psb[fb], btr[sl_t]], writes=[bfpr[ss]], out=fpr[ss],
                in0=c.bank(fb).rearrange("p (t k) -> p t k", k=128),
                in1=trB[sl_t][:, t0 + tq:t0 + tq + 4].unsqueeze(2).to_broadcast([128, 4, 128]), op=ALU.add)
            if c.simsafe:
                P.I("scalar", "activation", reads=[bfpr[ss]], writes=[bfpr[ss]], out=fpr[ss], in_=fpr[ss], func=AF.Relu)
            else:
                P.I("scalar", "activation", reads=[bfpr[ss]], writes=[bfpr[ss]], out=fpr[ss], in_=fpr[ss], func=AF.Prelu,
                    alpha=1.0e5)

        def front2(G):
            ss = G % 3
            P.I("scalar", "activation", reads=[bfpr[ss]], writes=[brr_[ss]], out=rr_[ss], in_=fpr[ss], func=AF.Exp)

        def back(G):
            i, s_, gq = groups[G]
            sl = (i * 4 + s_) % 2; tq = gq * 4
            wb = 3 + G % 2; ss = G % 3
            wsb, bwsb = wsb2[(i * 4 + s_) % 2], bwsb2[(i * 4 + s_) % 2]
            for tt in range(4):
                P.I("tensor", "matmul", reads=[brr_[ss], boh1[sl][gq]], writes=[c.psb[wb]],
                    out=c.bank(wb)[:, tt * 128:(tt + 1) * 128], lhsT=rr_[ss][:, tt, :], rhs=oh1[sl][:, tq + tt, :],
                    start=True, stop=True)
            P.I("scalar", "copy", reads=[c.psb[wb]], writes=[bwsb],
                out=wsb[:, :, tq:tq + 4, :].rearrange("p g t e -> p t g e"),
                in_=c.bank(wb).rearrange("p (t g e) -> p t g e", g=16, e=8))
            if gq == 7:
                P.D("sync", Wd[:, :, i * 128 + s_ * 32:i * 128 + s_ * 32 + 32, :], wsb, reads=[bwsb], writes=[bWd[i]])

        for _ in tile_prep(0):
            pass
        sub_prep(0, 0)
        NG = len(groups)
        gen = None
        hold = False
        for G in range(NG + 2):
            if G < NG:
                i, s_, gq = groups[G]
                if s_ < 3 and gq >= 2:
                    parts = [gq - 2] if gq < 6 else ([4, 5] if gq == 6 else [6, 7])
                    for pp in parts:
                        sub_prep(i, s_ + 1, pp)
                if i + 1 < ntiles:
                    new_tb = ((i + 1) % 4 == 0)
                    if gen is None and s_ == 0 and gq == 3:
                        gen = tile_prep(i + 1)
                        hold = False
                    if gen is not None and gen is not True:
                        if hold and s_ == 3:
                            hold = False
                        last = (s_ == 3 and gq == 3)
                        per_it = 7 if s_ < 3 else 30
                        if not hold:
                            try:
                                for _ in range(10 ** 9 if last else per_it):
                                    if next(gen) == "B" and new_tb and s_ < 3:
                                        hold = True
                                        break
                            except StopIteration:
                                gen = True
                    if s_ == 3 and gq >= 4:
                        for pp in (2 * (gq - 4), 2 * (gq - 4) + 1):
                            sub_prep(i + 1, 0, pp)
                        if gq == 7:
                            gen = None
                front(G)
            if G >= 2:
                back(G - 2)
            if G < NG:
                front2(G)
        if c.stop_after in ("Bi", "Bi1"):
            if "Wd" in c.debug:
                self.final.append(bWd[0].w)
            return
        P.barrier()

        o = 108 * K
        uTr = [c.carve(o + k * 16384, [8, 1024], BF16) for k in range(2)]; o += 32768; buTr = P.bufs(2, "uTr")
        vr = [c.carve(o + k * 16384, [8, 1024], BF16) for k in range(2)]; o += 32768; bvr = P.bufs(2, "vr")
        wc = [c.carve(o + k * 8192, [512, 8], BF16) for k in range(2)]; o += 16384; bwc = P.bufs(2, "wc")
        hh_ = [c.carve(o + k * 8192, [8, 512], BF16) for k in range(2)]; o += 16384
        bhh = [P.bufs(8, f"H{k}_") for k in range(2)]
        assert o <= SBUF_BYTES, o
        gg = [c.carve(10240 + k * 1024, [512], BF16) for k in range(2)]; bgg = P.bufs(2, "gg")
        ngroups = 16 if c.stop_after != "Bii1" else 1
        units = [(g, TB) for g in range(ngroups) for TB in range(4)]
        cnt = {"na": 0, "ny": 0}

        def a_chunk(u, cch):
            g, TB = units[u]
            us, bus = uTr[g % 2], buTr[g % 2]
            w_, bw_ = wc[u % 2], bwc[u % 2]
            if cch == 0:
                if TB == 0:
                    vs, bvs = vr[g % 2], bvr[g % 2]
                    P.D("gpsimd", us, dr["uT"][:, g * 1024:(g + 1) * 1024].rearrange("(kc p) e -> p kc e", p=128),
                        writes=[bus])
                    P.D("gpsimd", vs, dr["pv"][g * 1024:(g + 1) * 1024, :].rearrange("(cc p) d -> p cc d", p=128),
                        writes=[bvs])
                P.D("sync", w_, Wd[:, g, TB * 512:(TB + 1) * 512, :], reads=bWd[TB * 4:TB * 4 + 4], writes=[bw_])
            ab = cnt["na"] % 2; cnt["na"] += 1
            for kc in range(8):
                P.I("tensor", "matmul", reads=[bus, c.bxn2[TB]], writes=[c.psb[ab]], out=c.bank(ab),
                    lhsT=us[:, kc, cch * 128:(cch + 1) * 128], rhs=c.xn2T[:, kc, TB * 512:(TB + 1) * 512],
                    start=(kc == 0), stop=(kc == 7))
            P.I("scalar", "activation", reads=[c.psb[ab]], writes=[bgg[ab]], out=gg[ab], in_=c.bank(ab),
                func=AF.Gelu_apprx_tanh)
            P.I("vector", "tensor_tensor", reads=[bgg[ab], bw_], writes=[bhh[u % 2][cch]],
                out=hh_[u % 2][:, cch, :], in0=gg[ab], in1=w_[:, :, cch], op=ALU.mult)

        def y_group(u, k):
            g, TB = units[u]
            vs, bvs = vr[g % 2], bvr[g % 2]
            hf, tt = k // 4, k % 4
            yb = 2 + cnt["ny"] % 6; cnt["ny"] += 1
            for cch in range(8):
                P.I("tensor", "matmul", reads=[bhh[u % 2][cch], bvs], writes=[c.psb[yb]], out=c.bank(yb),
                    lhsT=hh_[u % 2][:, cch, tt * 128:(tt + 1) * 128], rhs=vs[:, cch, hf * 512:(hf + 1) * 512],
                    start=(cch == 0), stop=(cch == 7))
            ti = TB * 4 + tt
            P.I("vector", "tensor_tensor", reads=[c.psb[yb], bh[ti]], writes=[bh[ti]],
                out=h[:, ti, hf * 512:(hf + 1) * 512], in0=c.bank(yb), in1=h[:, ti, hf * 512:(hf + 1) * 512],
                op=ALU.add)

        for cch in range(8):
            a_chunk(0, cch)
        for u in range(len(units)):
            for k in range(8):
                y_group(u, k)
                if u + 1 < len(units):
                    a_chunk(u + 1, k)
        c.dump("h2", h, bh, [128, NT, D])

    def build_ple(self):
        P, c, nc = self.P, self, self.nc
        dr = self.dr
        K = 1024
        h, bh = c.h, c.bh
        xn3T = c.carve(12 * K, [8, T], BF16); bxn3 = P.bufs(4, "xn3")
        o = 108 * K
        wg = c.carve(o, [8, D], BF16); o += 16384; bwg = P.buf("wg")
        wp = c.carve(o, [2, D], BF16); o += 4096; bwp = P.buf("wp")
        pT = c.carve(o, [2, T], BF16); o += 8192; bpT = P.bufs(NT, "pT")
        fgb = c.carve(o, [D]); o += 4096; bfgb = P.buf("fgb")
        pt = [c.carve(o + k * 1024, [256]) for k in range(2)]; o += 2048; bpt = P.bufs(2, "pt")
        pbf = [c.carve(o + k * 512, [256], BF16) for k in range(2)]; o += 1024; bpbf = P.bufs(2, "pbf")
        sgt = [c.carve(o + k * 4096, [D]) for k in range(2)]; o += 8192; bsgt = P.bufs(2, "sgt")
        ot = [c.carve(o + k * 4096, [D]) for k in range(2)]; o += 8192; bot = P.bufs(2, "ot")
        junk = c.carve(o, [D], BF16); o += 2048; bjunk = P.buf("junk3")
        st3 = c.carve(o, [3, 16]); o += 192
        nscr = o
        P.D("gpsimd", wg, dr["wg"].rearrange("(kc p) n -> p kc n", p=128), writes=[bwg])
        P.D("gpsimd", wp, dr["wp"].rearrange("(kc p) n -> p kc n", p=128), writes=[bwp])
        P.D("sync", fgb, dr["fgb"], writes=[bfgb])
        c.rmsnorm_T(lambda i: (h[:, i, :], [bh[i]]), V_GPLE, xn3T, bxn3, nscr, [6, 7])
        for i in range(NT):
            P.D("sync", pt[i % 2], dr["p"][i * 128:(i + 1) * 128, :], writes=[bpt[i % 2]])
            P.I("vector", "tensor_copy", reads=[bpt[i % 2]], writes=[bpbf[i % 2]], out=pbf[i % 2], in_=pt[i % 2])
            tb_ = i % 2
            pst = c.bank(tb_, BF16).rearrange("p (a b) -> p a b", b=128)
            for k in range(2):
                P.I("tensor", "transpose", reads=[bpbf[i % 2], c.bcst], writes=[c.psb[tb_]], out=pst[:, k, :],
                    in_=pbf[i % 2][:, k * 128:(k + 1) * 128], identity=c.ident_bf)
            P.I("scalar", "copy", reads=[c.psb[tb_]], writes=[bpT[i]], out=pT[:, :, i * 128:(i + 1) * 128], in_=pst[:, 0:2, :])
        bout = P.bufs(2, "outt")

        def stage1(i):
            gb = (i % 2) * 4
            for hf in range(2):
                for kc in range(8):
                    P.I("tensor", "matmul", reads=[bwg, bxn3[i // 4]], writes=[c.psb[gb + hf]], out=c.bank(gb + hf),
                        lhsT=xn3T[:, kc, i * 128:(i + 1) * 128], rhs=wg[:, kc, hf * 512:(hf + 1) * 512], start=(kc == 0),
                        stop=(kc == 7))
            for hf in range(2):
                for kc in range(2):
                    P.I("tensor", "matmul", reads=[bwp, bpT[i]], writes=[c.psb[gb + 2 + hf]], out=c.bank(gb + 2 + hf),
                        lhsT=pT[:, kc, i * 128:(i + 1) * 128], rhs=wp[:, kc, hf * 512:(hf + 1) * 512], start=(kc == 0),
                        stop=(kc == 1))

        def stage2(i):
            gb = (i % 2) * 4
            P.I("scalar", "activation", reads=[c.psb[gb], c.psb[gb + 1]], writes=[bsgt[i % 2]], out=sgt[i % 2],
                in_=c.banks(gb, 2), func=AF.Sigmoid)
            P.I("vector", "tensor_tensor", reads=[bsgt[i % 2], c.psb[gb + 2], c.psb[gb + 3]], writes=[bsgt[i % 2]],
                out=sgt[i % 2], in0=sgt[i % 2], in1=c.banks(gb + 2, 2), op=ALU.mult)
            P.I("vector", "tensor_tensor", reads=[bsgt[i % 2], bh[i]], writes=[bh[i]], out=h[:, i, :], in0=sgt[i % 2],
                in1=h[:, i, :], op=ALU.add)

        def stage3(i):
            bst = P.buf("fstat")
            P.I("scalar", "activation", reads=[bh[i]], writes=[bjunk, bst], out=junk, in_=h[:, i, :], func=AF.Square,
                accum_out=st3[:, 0, i:i + 1])
            P.I("scalar", "activation", reads=[bst], writes=[bst], out=st3[:, 1, i:i + 1], in_=st3[:, 0, i:i + 1],
                func=AF.Sqrt, scale=1.0 / D, bias=c.epsc)
            P.I("vector", "reciprocal", reads=[bst], writes=[bst], out=st3[:, 2, i:i + 1], in_=st3[:, 1, i:i + 1])
            P.I("vector", "scalar_tensor_tensor", reads=[bh[i], bst, bfgb], writes=[bot[i % 2]], out=ot[i % 2], in0=h[:, i, :],
                scalar=st3[:, 2, i:i + 1], in1=fgb, op0=ALU.mult, op1=ALU.mult)
            od = P.D("sync", self.out[i * 128:(i + 1) * 128, :], ot[i % 2], reads=[bot[i % 2]], writes=[bout[i % 2]])
            self.final.append(od)

        for i in range(NT + 1):
            if i < NT:
                stage1(i)
            if i >= 1:
                stage2(i - 1)
        for i in range(NT):
            stage3(i)

    def finish(self):
        self.P.finish(final_waits=self.final)
        return self.nc


def kernel(**inputs):
    nb = 8
    sh = _shared_inputs(inputs)
    x = np.asarray(inputs["x"], np.float32)
    p = np.asarray(inputs["p"], np.float32)
    b = Builder()
    nc = b.build()
    in_maps = []
    for i in range(nb):
        m = dict(sh)
        m["x"] = np.ascontiguousarray(x[i])
        m["p"] = np.ascontiguousarray(p[0, i])
        in_maps.append({k: v for k, v in m.items() if k in b.dr})
    res = run_bass_kernel_spmd(nc, in_maps, core_ids=list(range(nb)))
    return np.stack([np.asarray(res.results[i]["out"], np.float32) for i in range(nb)], axis=0)
```
